# Optimizing a Trainium2 kernel written in Bass

```python
import jax, jax.numpy as jnp
from jax import lax
import numpy as np

D_MODEL = 1024
BATCH = 4
SEQ = 8192
DEPTH = 1

RET_HEADS = 4
RET_QK_DIM = 128
RET_V_DIM = 256
RET_CHUNK = 128
ROPE_BASE = 10000.0
SWA_Q_HEADS = 8
SWA_KV_HEADS = 2
SWA_HEAD_DIM = 64
SWA_WINDOW = 128
MOE_GROUPS = 4
MOE_EXPERTS_PER_GROUP = 8
MOE_TOP_K = 2
MOE_D_FF = 512
MOE_BLOCK = 128
NORM_EPS = 1e-6

RET_QK_W = RET_HEADS * RET_QK_DIM
RET_V_W = RET_HEADS * RET_V_DIM
SWA_Q_W = SWA_Q_HEADS * SWA_HEAD_DIM
SWA_KV_W = SWA_KV_HEADS * SWA_HEAD_DIM
IN_SPLITS = (RET_QK_W, RET_QK_W, RET_V_W, RET_V_W, SWA_Q_W, SWA_KV_W, SWA_KV_W, D_MODEL, D_MODEL)
IN_WIDTH = int(sum(IN_SPLITS))
IN_CUTS = tuple(int(c) for c in np.cumsum(IN_SPLITS)[:-1])
MOE_N_EXPERTS = MOE_GROUPS * MOE_EXPERTS_PER_GROUP

kernel_name = "hybrid_retention_swa_sink_hmoe_block"


def rmsnorm(x, g):
    x32 = x.astype(jnp.float32)
    r = lax.rsqrt(jnp.mean(x32 * x32, axis=-1, keepdims=True) + NORM_EPS)
    return (x32 * r).astype(x.dtype) * g


def rotary(x, pos):
    half = x.shape[-1] // 2
    inv = ROPE_BASE ** (-jnp.arange(half, dtype=jnp.float32) / half)
    ang = pos.astype(jnp.float32)[:, None] * inv[None, :]
    cos = jnp.cos(ang)[None, :, None, :]
    sin = jnp.sin(ang)[None, :, None, :]
    x1, x2 = x[..., :half], x[..., half:]
    return jnp.concatenate([x1 * cos - x2 * sin, x1 * sin + x2 * cos], axis=-1)


def retention_chunkwise(q, k, v):
    B, S, H, dk = q.shape
    dv = v.shape[-1]
    C = RET_CHUNK
    nC = S // C
    q = q.reshape(B, nC, C, H, dk)
    k = k.reshape(B, nC, C, H, dk) * (dk ** -0.5)
    v = v.reshape(B, nC, C, H, dv)
    log_g = jnp.log1p(-jnp.exp2(-5.0 - jnp.arange(H, dtype=jnp.float32)))
    idx = jnp.arange(C, dtype=jnp.float32)
    diff = idx[:, None] - idx[None, :]
    decay = jnp.where(diff >= 0, jnp.exp(log_g[:, None, None] * jnp.maximum(diff, 0.0)), 0.0)
    scores = jnp.einsum('bcqhd,bckhd->bchqk', q, k) * decay[None, None]
    intra = jnp.einsum('bchqk,bckhe->bcqhe', scores, v)
    k_dec = k * jnp.exp(log_g[None, :] * (C - 1 - idx)[:, None])[None, None, :, :, None]
    kv = jnp.einsum('bckhd,bckhe->cbhde', k_dec, v)
    chunk_decay = jnp.exp(log_g * C)[None, :, None, None]

    def step(R, kv_c):
        return chunk_decay * R + kv_c, R

    _, R_prev = lax.scan(step, jnp.zeros_like(kv[0]), kv)
    q_dec = q * jnp.exp(log_g[None, :] * (idx + 1.0)[:, None])[None, None, :, :, None]
    cross = jnp.einsum('bcqhd,cbhde->bcqhe', q_dec, R_prev)
    return (intra + cross).reshape(B, S, H, dv)


def swa_sink_attention(q, k, v, sinks):
    B, S, Hq, d = q.shape
    Hkv = k.shape[2]
    G = Hq // Hkv
    W = SWA_WINDOW
    nB = S // W
    qb = q.reshape(B, nB, W, Hkv, G, d)
    kb = k.reshape(B, nB, W, Hkv, d)
    vb = v.reshape(B, nB, W, Hkv, d)
    pad = jnp.zeros_like(kb[:, :1])
    k_ext = jnp.concatenate([jnp.concatenate([pad, kb[:, :-1]], axis=1), kb], axis=2)
    v_ext = jnp.concatenate([jnp.concatenate([pad, vb[:, :-1]], axis=1), vb], axis=2)
    s = jnp.einsum('bnqkgd,bnskd->bkgnqs', qb, k_ext).astype(jnp.float32) * (d ** -0.5)
    blk = jnp.arange(nB)[:, None, None]
    qpos = blk * W + jnp.arange(W)[None, :, None]
    kpos = (blk - 1) * W + jnp.arange(2 * W)[None, None, :]
    rel = qpos - kpos
    valid = (rel >= 0) & (rel < W) & (kpos >= 0)
    s = jnp.where(valid, s, -jnp.inf)
    sink = sinks.astype(jnp.float32).reshape(Hkv, G)[None, :, :, None, None, None]
    m = jnp.maximum(jnp.max(s, axis=-1, keepdims=True), sink)
    p = jnp.exp(s - m)
    denom = jnp.sum(p, axis=-1, keepdims=True) + jnp.exp(sink - m)
    p = (p / denom).astype(v.dtype)
    o = jnp.einsum('bkgnqs,bnskd->bnqkgd', p, v_ext)
    return o.reshape(B, S, Hq * d)


def hierarchical_moe(h, w_rg, b_rg, w_re, b_re, w_gate, w_up, w_down):
    N, D = h.shape
    E = MOE_EXPERTS_PER_GROUP
    K = MOE_TOP_K
    BLK = MOE_BLOCK
    group_probs = jax.nn.softmax((h @ w_rg).astype(jnp.float32) + b_rg, axis=-1)
    g_prob, g_idx = lax.top_k(group_probs, 1)
    e_logits = ((h @ w_re).astype(jnp.float32) + b_re).reshape(N, MOE_GROUPS, E)
    sel = jnp.take_along_axis(e_logits, g_idx[:, :, None], axis=1)[:, 0]
    top_logit, e_idx = lax.top_k(sel, K)
    e_w = jax.nn.softmax(top_logit, axis=-1) * g_prob
    expert_id = g_idx * E + e_idx
    A = N * K
    flat_e = expert_id.reshape(A)
    flat_tok = jnp.repeat(jnp.arange(N, dtype=jnp.int32), K)
    flat_w = e_w.reshape(A)
    order = jnp.argsort(flat_e)
    se, stok, sw = flat_e[order], flat_tok[order], flat_w[order]
    counts = jnp.zeros((MOE_N_EXPERTS,), jnp.int32).at[flat_e].add(1)
    starts = jnp.cumsum(counts) - counts
    padded = (counts + BLK - 1) // BLK * BLK
    pends = jnp.cumsum(padded)
    pstarts = pends - padded
    dest = pstarts[se] + jnp.arange(A, dtype=jnp.int32) - starts[se]
    P = A + MOE_N_EXPERTS * BLK
    n_blk = P // BLK
    row_tok = jnp.zeros((P,), jnp.int32).at[dest].set(stok)
    row_w = jnp.zeros((P,), h.dtype).at[dest].set(sw.astype(h.dtype))
    blk_expert = jnp.minimum(jnp.searchsorted(pends, jnp.arange(n_blk, dtype=jnp.int32) * BLK, side='right'),
                             MOE_N_EXPERTS - 1)
    xs = h[row_tok].reshape(n_blk, BLK, D)

    def expert_block(args):
        xb, e = args
        return (jax.nn.silu(xb @ w_gate[e]) * (xb @ w_up[e])) @ w_down[e]

    ys = lax.map(expert_block, (xs, blk_expert)).reshape(P, D)
    return jnp.zeros_like(h).at[row_tok].add(ys * row_w[:, None])


def setup_inputs(seed: int = 0) -> dict:
    key = jax.random.key(seed)
    ks = jax.random.split(key, 20)
    L, D = DEPTH, D_MODEL
    f32 = jnp.float32

    def nrm(k, shape, scale):
        return jax.random.normal(k, shape, f32) * scale

    return {
        "x": nrm(ks[0], (BATCH, SEQ, D), 1.0),
        "norm_mix_g": 1.0 + nrm(ks[1], (L, D), 0.02),
        "w_in": nrm(ks[2], (L, D, IN_WIDTH), D ** -0.5),
        "ret_gn_g": 1.0 + nrm(ks[3], (L, RET_HEADS, RET_V_DIM), 0.02),
        "w_ret_o": nrm(ks[4], (L, RET_V_W, D), RET_V_W ** -0.5),
        "q_norm_g": 1.0 + nrm(ks[5], (L, SWA_HEAD_DIM), 0.02),
        "k_norm_g": 1.0 + nrm(ks[6], (L, SWA_HEAD_DIM), 0.02),
        "sinks": nrm(ks[7], (L, SWA_Q_HEADS), 0.5),
        "w_swa_o": nrm(ks[8], (L, SWA_Q_W, D), SWA_Q_W ** -0.5),
        "w_out": nrm(ks[9], (L, D, D), D ** -0.5),
        "norm_ffn_g": 1.0 + nrm(ks[10], (L, D), 0.02),
        "w_router_group": nrm(ks[11], (L, D, MOE_GROUPS), D ** -0.5),
        "b_router_group": nrm(ks[12], (L, MOE_GROUPS), 0.01),
        "w_router_expert": nrm(ks[13], (L, D, MOE_N_EXPERTS), D ** -0.5),
        "b_router_expert": nrm(ks[14], (L, MOE_N_EXPERTS), 0.01),
        "w_gate": nrm(ks[15], (L, MOE_N_EXPERTS, D, MOE_D_FF), D ** -0.5),
        "w_up": nrm(ks[16], (L, MOE_N_EXPERTS, D, MOE_D_FF), D ** -0.5),
        "w_down": nrm(ks[17], (L, MOE_N_EXPERTS, MOE_D_FF, D), MOE_D_FF ** -0.5),
    }


def reference(x, norm_mix_g, w_in, ret_gn_g, w_ret_o, q_norm_g, k_norm_g, sinks, w_swa_o, w_out,
              norm_ffn_g, w_router_group, b_router_group, w_router_expert, b_router_expert,
              w_gate, w_up, w_down):
    B, S, D = x.shape
    pos = jnp.arange(S, dtype=jnp.int32)
    for l in range(DEPTH):
        h = rmsnorm(x, norm_mix_g[l])
        proj = h @ w_in[l]
        rq, rk, rv, rg, sq, sk, sv, gate_a, gate_b = jnp.split(proj, IN_CUTS, axis=-1)
        rq = rotary(rq.astype(jnp.float32).reshape(B, S, RET_HEADS, RET_QK_DIM), pos)
        rk = rotary(rk.astype(jnp.float32).reshape(B, S, RET_HEADS, RET_QK_DIM), pos)
        rv = rv.astype(jnp.float32).reshape(B, S, RET_HEADS, RET_V_DIM)
        ret = retention_chunkwise(rq, rk, rv)
        mu = jnp.mean(ret, axis=-1, keepdims=True)
        var = jnp.mean(jnp.square(ret - mu), axis=-1, keepdims=True)
        ret = ((ret - mu) * lax.rsqrt(var + NORM_EPS)).astype(x.dtype) * ret_gn_g[l]
        ret = jax.nn.silu(rg) * ret.reshape(B, S, RET_V_W)
        y_a = ret @ w_ret_o[l]
        sq = rmsnorm(sq.reshape(B, S, SWA_Q_HEADS, SWA_HEAD_DIM), q_norm_g[l])
        sk = rmsnorm(sk.reshape(B, S, SWA_KV_HEADS, SWA_HEAD_DIM), k_norm_g[l])
        sv = sv.reshape(B, S, SWA_KV_HEADS, SWA_HEAD_DIM)
        y_b = swa_sink_attention(sq, sk, sv, sinks[l]) @ w_swa_o[l]
        merged = jax.nn.sigmoid(gate_a) * y_a + jax.nn.sigmoid(gate_b) * y_b
        x = x + merged @ w_out[l]
        h2 = rmsnorm(x, norm_ffn_g[l]).reshape(B * S, D)
        moe = hierarchical_moe(h2, w_router_group[l], b_router_group[l], w_router_expert[l],
                               b_router_expert[l], w_gate[l], w_up[l], w_down[l])
        x = x + moe.reshape(B, S, D)
    return x
```

```python
import contextlib
import numpy as np
import concourse.bass as bass
import concourse.mybir as mybir
from concourse.bass_utils import run_bass_kernel_spmd

F32 = mybir.dt.float32
BF16 = mybir.dt.bfloat16
I32 = mybir.dt.int32
AF = mybir.ActivationFunctionType
ALU = mybir.AluOpType
AX = mybir.AxisListType

D = 1024
NH = 4
DK = 128
DV = 256
INW = 5888
NE = 32
DFF = 512
EPS = 1e-6
O_RQ, O_RK, O_RV, O_RG, O_SQ, O_SK, O_SV, O_GA, O_GB = 0, 512, 1024, 2048, 3072, 3584, 3712, 3840, 4864

C_IDENT = 0
C_DTP = 128
C_DQROW = 640
C_U = 1152
C_KDEC = 1280
C_EC = 1284
C_GMIX = 1316
C_GN = 1324
C_GFFN = 1332
C_GQ = 1340
C_GK = 1404
C_SINK = 1468
C_BR = 1476
C_GFFNBC = 1512
B_IDENT, B_MASKC, B_MASKP, B_MASKP0 = 0, 128, 256, 384
NCST = C_GFFNBC


class Tok:
    __slots__ = ("sem", "val")

    def __init__(self, sem, val):
        self.sem = sem
        self.val = val


class Buf:
    __slots__ = ("name", "w", "r", "lock")

    def __init__(self, name):
        self.name = name
        self.w = None
        self.r = {}
        self.lock = None


class Eng:
    def __init__(self, name, sem):
        self.name = name
        self.sem = sem
        self.n = 0
        self.known = {}
        self.prog = []


class Stream:
    def __init__(self, sem):
        self.sem = sem
        self.count = 0


class Sched:
    def __init__(self, nc, stack):
        self.nc = nc
        self.stack = stack
        self.eng = {}
        for nm in ("pe", "act", "dve", "pool", "sp"):
            self.eng[nm] = Eng(nm, stack.enter_context(nc.semaphore("s_" + nm)))
        self.streams = []

    def stream(self, name):
        s = Stream(self.stack.enter_context(self.nc.semaphore("d_" + name)))
        self.streams.append(s)
        return s

    def _deps(self, E, reads, writes):
        deps = []
        for b in reads:
            if b.w is not None:
                deps.append((b.w, True))
        for b in writes:
            if b.w is not None:
                deps.append((b.w, False))
            for t in b.r.values():
                deps.append((t, False))
        for tok, raw in deps:
            if tok.sem is E.sem:
                if E.name == "pe":
                    continue
                assert tok.val <= E.n, "same-engine dependency on unsignaled op"
            if E.known.get(id(tok.sem), 0) >= tok.val:
                continue
            E.prog.append(("wait", tok.sem, tok.val))
            E.known[id(tok.sem)] = tok.val

    def _mark(self, tok, reads, writes):
        for b in writes:
            b.w = tok
            b.r = {}
        for b in reads:
            old = b.r.get(id(tok.sem))
            if old is None or old.val < tok.val:
                b.r[id(tok.sem)] = tok

    def op(self, e, fn, reads=(), writes=(), signal=True):
        E = self.eng[e]
        if e != "pe":
            locks = [b.lock for b in reads if b.lock is not None]
            if locks:
                writes = list(writes) + locks
        self._deps(E, reads, writes)
        if signal:
            E.n += 1
            tok = Tok(E.sem, E.n)
        else:
            tok = Tok(E.sem, E.n + 1)
        E.prog.append(("op", fn, signal))
        self._mark(tok, reads, writes)
        return tok

    def dma(self, q, st, fn, reads=(), writes=()):
        E = self.eng[q]
        self._deps(E, reads, writes)
        st.count += 16
        tok = Tok(st.sem, st.count)
        E.prog.append(("dma", fn, st.sem))
        self._mark(tok, reads, writes)
        return tok

    def seal(self, st, bufs):
        for b in bufs:
            b.w = Tok(st.sem, st.count)

    def barrier(self):
        toks = [Tok(E.sem, E.n) for E in self.eng.values() if E.n > 0]
        toks += [Tok(s.sem, s.count) for s in self.streams if s.count > 0]
        for E in self.eng.values():
            for t in toks:
                if t.sem is E.sem:
                    continue
                if E.known.get(id(t.sem), 0) >= t.val:
                    continue
                E.prog.append(("wait", t.sem, t.val))
                E.known[id(t.sem)] = t.val

    def emit(self):
        nc = self.nc
        sch = self

        def run(engobj, E):
            for item in E.prog:
                if item[0] == "wait":
                    engobj.wait_ge(item[1], item[2])
                elif item[0] == "op":
                    ins = item[1](engobj)
                    if item[2]:
                        ins.then_inc(E.sem, 1)
                else:
                    ins = item[1](engobj)
                    ins.then_inc(item[2], 16)

        with nc.Block() as blk:
            @blk.tensor
            def _(e):
                run(e, sch.eng["pe"])

            @blk.scalar
            def _(e):
                run(e, sch.eng["act"])

            @blk.vector
            def _(e):
                run(e, sch.eng["dve"])

            @blk.gpsimd
            def _(e):
                run(e, sch.eng["pool"])

            @blk.sync
            def _(e):
                run(e, sch.eng["sp"])


class T:
    def __init__(self, t, name):
        self.t = t
        self.b = Buf(name)

    def __getitem__(self, k):
        return self.t[k]


def build(NPREV=32, NOWN=32, CAP=384, moe=True, dbg_stop=99):
    nc = bass.Bass("TRN2", target_bir_lowering=False)
    NT = NPREV + NOWN
    NTOK = NOWN * 128
    NSLOT = NE * CAP
    XS_ROWS = NSLOT + 128

    def din(name, shape, dt=F32):
        return nc.dram_tensor(name, list(shape), dt, kind="ExternalInput").ap()

    xc = din("xc", [NT * 128, D])
    cs = din("cs", [NT * 128, 128])
    cst = din("cst", [128, NCST])
    cstb = din("cstb", [128, 512])
    gffn = din("gffn", [1, D])
    w_in = din("w_in", [D, INW])
    w_ret_o = din("w_ret_o", [D, D])
    w_swa_o = din("w_swa_o", [512, D])
    w_out = din("w_out", [D, D])
    wr = din("wr", [D, 36])
    w_gate = din("w_gate", [NE, D, DFF])
    w_up = din("w_up", [NE, D, DFF])
    w_down = din("w_down", [NE, DFF, D])
    y = nc.dram_tensor("y", [NTOK, D], F32, kind="ExternalOutput").ap()
    x1buf = nc.dram_tensor("x1buf", [NTOK, D], F32, kind="Internal").ap()
    h2buf = nc.dram_tensor("h2buf", [NTOK, D], BF16, kind="Internal").ap()
    xsbuf = nc.dram_tensor("xsbuf", [XS_ROWS, D], BF16, kind="Internal").ap()
    ysbuf = nc.dram_tensor("ysbuf", [XS_ROWS, D], BF16, kind="Internal").ap()

    gam = [1.0 - 2.0 ** (-5.0 - h) for h in range(NH)]
    cdec = [g ** 128 for g in gam]

    with contextlib.ExitStack() as stack:
        S = Sched(nc, stack)

        def sb(name, shape, dt):
            return T(stack.enter_context(nc.sbuf_tensor("sb_" + name, list(shape), dt)), name)

        banks = [T(stack.enter_context(nc.psum_tensor("ps%d" % i, [128, 512], F32)), "ps%d" % i)
                 for i in range(8)]
        for bk in banks:
            bk.b.lock = Buf(bk.b.name + "_lock")
        bank_i = [0]

        ring_i = {"f": 0, "b": 0}

        def psum(th=None):
            if th is None:
                b = banks[bank_i[0] % 8]
                bank_i[0] += 1
                return b
            assert free_banks, "PSUM pool exhausted"
            b = free_banks.pop(0)
            held[th].append(b)
            return b

        free_banks = list(banks)
        held = {"f": [], "b": []}

        def release(th):
            free_banks.extend(held[th])
            held[th] = []

        C = sb("cst", [128, NCST], F32)
        CB = sb("cstb", [128, 512], BF16)

        class _View:
            def __init__(self, t, off, b):
                self.t, self.off, self.b = t, off, b

            def __getitem__(self, k):
                return self.t[:, self.off:self.off + 128][k]

        identb = _View(CB, B_IDENT, CB.b)
        maskC = _View(CB, B_MASKC, CB.b)
        maskP = _View(CB, B_MASKP, CB.b)
        maskP0 = _View(CB, B_MASKP0, CB.b)
        ones_b = sb("ones_b", [128, 128], BF16)
        gqk = sb("gqk", [128, 64], F32)
        negc = sb("negc", [128, 1], F32)
        sinkexp = sb("sinkexp", [128, 8], F32)
        wrs = sb("wrs", [128, 8, 36], F32)
        L = sb("L", [128, NOWN, 36], F32)
        small = sb("small", [128, 64], F32)
        nhalf = sb("nhalf", [128, 8], F32)
        S.op("pool", lambda e: e.memset(nhalf[:, :], -0.5), [], [nhalf.b])

        st_c = S.stream("cst")
        S.dma("sp", st_c, lambda e: e.dma_start(out=C[:, :], in_=cst), writes=[C.b])
        S.dma("sp", st_c, lambda e: e.dma_start(out=wrs[:, :, :], in_=wr.rearrange("(k p) n -> p k n", p=128)),
              writes=[wrs.b])
        S.seal(st_c, [C.b, wrs.b])
        st_cb = S.stream("cstb")
        S.dma("pool", st_cb, lambda e: e.dma_start(out=CB[:, :], in_=cstb), writes=[CB.b])
        S.op("dve", lambda e: e.memset(ones_b[:, :], 1.0), [], [ones_b.b])
        S.op("dve", lambda e: e.scalar_tensor_tensor(out=gqk[:, :], in0=C[:, C_GQ:C_GQ + 64], scalar=0.125,
                                                     in1=C[:, C_GK:C_GK + 64], op0=ALU.mult, op1=ALU.mult),
             [C.b], [gqk.b])
        S.op("dve", lambda e: e.tensor_reduce(out=small[:, 0:1], in_=gqk[:, :], axis=AX.X, op=ALU.max,
                                              apply_absolute_value=True), [gqk.b], [small.b])
        S.op("dve", lambda e: e.tensor_scalar(out=negc[:, :], in0=small[:, 0:1], scalar1=-64.0, scalar2=None,
                                              op0=ALU.mult), [small.b], [negc.b])
        S.op("act", lambda e: e.activation(out=sinkexp[:, :], in_=C[:, C_SINK:C_SINK + 8], func=AF.Exp,
                                           bias=negc[:, 0:1], scale=1.0), [C.b, negc.b], [sinkexp.b])
        for k in range(8):
            S.op("dve", lambda e, k=k: e.tensor_scalar(out=wrs[:, k, :], in0=wrs[:, k, :],
                                                       scalar1=C[:, C_GFFN + k:C_GFFN + k + 1], scalar2=None,
                                                       op0=ALU.mult), [C.b, wrs.b], [wrs.b])

        with contextlib.ExitStack() as p1:
            def sb1(name, shape, dt):
                return T(p1.enter_context(nc.sbuf_tensor("sb_" + name, list(shape), dt)), name)

            class TV:
                def __init__(self, ap, b):
                    self.ap, self.b = ap, b

                def __getitem__(self, k):
                    return self.ap[k]

            Win = sb1("Win", [128, 8, INW], BF16)
            Wro = sb1("Wro", [128, 8, D], BF16)
            Wso = sb1("Wso", [128, 4, D], BF16)
            Wout = sb1("Wout", [128, 8, D], BF16)
            WA0, WA1 = O_RK, O_RG
            WinA = Buf("WinA")
            WinB = Buf("WinB")
            st_wa = S.stream("w1a")
            st_w = S.stream("w1")
            for k in range(8):
                S.dma("pool", st_wa, lambda e, k=k: e.dma_start(out=Win[:, k, WA0:WA1],
                                                                  in_=w_in[k * 128:(k + 1) * 128, WA0:WA1]),
                      writes=[WinA])
            S.seal(st_wa, [WinA])
            wq = []
            for k in range(8):
                wq.append(lambda k=k: S.dma("pool", st_w, lambda e: e.dma_start(
                    out=Win[:, k, 0:WA0], in_=w_in[k * 128:(k + 1) * 128, 0:WA0]), writes=[WinB]))
                wq.append(lambda k=k: S.dma("pool", st_w, lambda e: e.dma_start(
                    out=Win[:, k, WA1:INW], in_=w_in[k * 128:(k + 1) * 128, WA1:INW], max_dma_last_dim=7680),
                    writes=[WinB]))
            wq.append(lambda: S.dma("pool", st_w, lambda e: e.dma_start(
                out=Wro[:, :, :], in_=w_ret_o.rearrange("(k p) n -> p k n", p=128)), writes=[Wro.b]))
            wq.append(lambda: S.dma("pool", st_w, lambda e: e.dma_start(
                out=Wso[:, :, :], in_=w_swa_o.rearrange("(k p) n -> p k n", p=128)), writes=[Wso.b]))
            wq.append(lambda: S.dma("pool", st_w, lambda e: e.dma_start(
                out=Wout[:, :, :], in_=w_out.rearrange("(k p) n -> p k n", p=128)), writes=[Wout.b]))

            def issue_weights(n):
                for _ in range(n):
                    if wq:
                        wq.pop(0)()
                if not wq:
                    S.seal(st_w, [WinB, Wro.b, Wso.b, Wout.b])
            S.op("dve", lambda e: e.tensor_scalar(out=small[:, 8:16], in0=C[:, C_GN:C_GN + 8], scalar1=0.5,
                                                  scalar2=None, op0=ALU.mult), [C.b], [small.b])

            def fold_win(c0, c1, wb):
                for k in range(8):
                    if k % 2 == 0:
                        S.op("dve", lambda e, k=k: e.tensor_scalar(out=Win[:, k, c0:c1], in0=Win[:, k, c0:c1],
                                                                   scalar1=C[:, C_GMIX + k:C_GMIX + k + 1], scalar2=None,
                                                                   op0=ALU.mult), [C.b, wb], [wb])
                    else:
                        S.op("act", lambda e, k=k: e.activation(out=Win[:, k, c0:c1], in_=Win[:, k, c0:c1], func=AF.Copy,
                                                                scale=C[:, C_GMIX + k:C_GMIX + k + 1]), [C.b, wb], [wb])

            fold_win(WA0, WA1, WinA)

            def fold_rest():
                fold_win(0, WA0, WinB)
                fold_win(WA1, INW, WinB)
                for k in range(8):
                    if k % 2 == 1:
                        S.op("dve", lambda e, k=k: e.tensor_scalar(out=Wro[:, k, :], in0=Wro[:, k, :],
                                                                   scalar1=small[:, 8 + k:9 + k], scalar2=None,
                                                                   op0=ALU.mult), [small.b, Wro.b], [Wro.b])
                    else:
                        S.op("act", lambda e, k=k: e.activation(out=Wro[:, k, :], in_=Wro[:, k, :], func=AF.Copy,
                                                                scale=small[:, 8 + k:9 + k]), [small.b, Wro.b], [Wro.b])
                S.op("dve", lambda e: e.tensor_scalar(out=Wout[:, 0:4, :], in0=Wout[:, 0:4, :], scalar1=0.5, scalar2=None,
                                                      op0=ALU.mult), [Wout.b], [Wout.b])
                S.op("act", lambda e: e.activation(out=Wout[:, 4:8, :], in_=Wout[:, 4:8, :], func=AF.Copy, scale=0.5),
                     [Wout.b], [Wout.b])

            xt = [sb1("xt%d" % i, [128, D], F32) for i in range(2)]
            cst_t = [sb1("cs%d" % i, [128, 128], F32) for i in range(2)]
            st_x = [S.stream("x%d" % i) for i in range(2)]
            h = sb1("h", [128, D], BF16)
            qrot = sb1("qrot", [128, 4, 2, 64], BF16)
            krot = sb1("krot", [128, 4, 2, 64], BF16)
            rt = [sb1("rt%d" % i, [128, 4, 64], BF16) for i in range(4)]
            v = sb1("v", [128, D], BF16)
            sgt = [sb1("sgt%d" % i, [128, 512], BF16) for i in range(2)]
            qT = sb1("qT", [128, 4, 128], BF16)
            kT = sb1("kT", [128, 4, 128], BF16)
            kdec = sb1("kdec", [128, 4, 128], BF16)
            SdT = sb1("SdT", [128, 4, 128], BF16)
            R = sb1("R", [128, 4, DV], F32)
            Rb = sb1("Rb", [128, 4, DV], BF16)
            gnt = sb1("gnt", [128, D], BF16)
            qn = sb1("qn", [128, 4, 2, 64], BF16)
            kn = sb1("kn", [128, 128], BF16)
            qnT = sb1("qnT", [128, 4, 128], BF16)
            sqk = kn
            sqq = TV(qn.t[:, :, :, :].rearrange("p i j d -> p (i j d)"), qn.b)
            kTr = [sb1("kTr%d" % i, [128, 128], BF16) for i in range(3)]
            vsr = [sb1("vsr%d" % i, [128, 2, 65], BF16) for i in range(3)]
            eb = [sb1("eb%d" % i, [128, 512], BF16) for i in range(4)]
            on = sb1("on", [128, 8, 64], BF16)
            stat = sb1("stat", [128, 64], F32)
            statp = sb1("statp", [128, 32], F32)
            hT2 = [sb1("hT%d" % i, [128, 8, 128], BF16) for i in range(2)]
            ret2 = [sb1("ret%d" % i, [128, D], BF16) for i in range(2)]
            oTn1 = sb1("oTn", [128, 4, 128], BF16)
            oTn2 = [oTn1, oTn1]
            xT2 = sb1("xT2", [128, 8, 128], BF16)
            ma = sb1("ma", [128, D], BF16)
            tb = sb1("tb", [128, D], BF16)
            sgb = [sb1("sgb%d" % i, [128, 512], BF16) for i in range(4)]
            x1T = sb1("x1T", [128, 4, 128], F32)
            statb = sb1("statb", [128, 48], F32)
            rr2 = [sb1("rr%d" % i, [128, 2], F32) for i in range(2)]
            h2 = ma
            eb_i = [0]
            sg_i = [0]
            sgb_i = [0]

            S.op("dve", lambda e: e.memset(R[:, :, :], 0.0), [], [R.b])
            S.op("pool", lambda e: e.memset(Rb[:, :, :], 0.0), [], [Rb.b])
            for i_ in range(3):
                S.op("pool", lambda e, i_=i_: e.memset(vsr[i_][:, :, :], 1.0), [], [vsr[i_].b])

            st_z = S.stream("zfill")

            def zero_fill():
                if not moe:
                    return
                S.op("pool", lambda e: e.memset(gnt[:, :], 0.0), [], [gnt.b])
                nblk = XS_ROWS // 128
                for b0 in range(0, nblk, 16):
                    nb = min(16, nblk - b0)
                    S.dma("sp", st_z, lambda e, b0=b0, nb=nb: e.dma_start(
                        out=xsbuf[b0 * 128:(b0 + nb) * 128, :].rearrange("(n p) d -> p n d", p=128),
                        in_=gnt[:, :].unsqueeze(1).broadcast_to([128, nb, D])), reads=[gnt.b])

            st_x1 = [S.stream("x1st%d" % i) for i in range(2)]
            st_h2 = S.stream("h2st")
            x1_tiles = [Buf("x1d%d" % j) for j in range(NOWN)]
            h2_tiles = [Buf("h2d%d" % j) for j in range(NOWN)]

            def load_tile(ci):
                s = ci % 2
                S.dma("sp", st_x[s], lambda e: e.dma_start(out=xt[s][:, :], in_=xc[ci * 128:(ci + 1) * 128, :]),
                      writes=[xt[s].b])
                S.dma("sp", st_x[s], lambda e: e.dma_start(out=cst_t[s][:, :], in_=cs[ci * 128:(ci + 1) * 128, :]),
                      writes=[cst_t[s].b])
                S.seal(st_x[s], [xt[s].b, cst_t[s].b])

            def inproj(th, hT, col0, ncol):
                ps = psum(th)
                for k in range(8):
                    S.op("pe", lambda e, k=k, ps=ps: e.matmul(ps[:, 0:ncol], lhsT=hT[:, k, :],
                                                              rhs=Win[:, k, col0:col0 + ncol],
                                                              start=(k == 0), stop=(k == 7)),
                         [hT.b, WinA if WA0 <= col0 < WA1 else WinB], [ps.b], signal=(k == 7))
                return ps

            def transposes(src, nblk, ps, rbufs, dst_off=0, dt=BF16, ident=None):
                ident = ident or identb
                pv = ps[:, :].bitcast(dt) if dt == BF16 else ps[:, :]
                for k in range(nblk):
                    S.op("pe", lambda e, k=k: e.transpose(pv[:, dst_off + k * 128:dst_off + (k + 1) * 128],
                                                          src(k), ident[:, :]),
                         list(rbufs) + [ident.b], [ps.b], signal=(k == nblk - 1))

            def rotary(ps, cst_s, dst, rt, r_ap, rb):
                pv = ps[:, :].rearrange("p (h t d) -> p h t d", h=4, t=2, d=64)
                cosb = cst_s[:, 0:64].unsqueeze(1).broadcast_to([128, 4, 64])
                sinb = cst_s[:, 64:128].unsqueeze(1).broadcast_to([128, 4, 64])
                S.op("dve", lambda e: e.scalar_tensor_tensor(out=rt[0][:, :, :], in0=pv[:, :, 0, :], scalar=r_ap,
                                                             in1=cosb, op0=ALU.mult, op1=ALU.mult),
                     [ps.b, cst_s.b, rb], [rt[0].b])
                S.op("dve", lambda e: e.scalar_tensor_tensor(out=rt[1][:, :, :], in0=pv[:, :, 1, :], scalar=r_ap,
                                                             in1=sinb, op0=ALU.mult, op1=ALU.mult),
                     [ps.b, cst_s.b, rb], [rt[1].b])
                S.op("dve", lambda e: e.tensor_tensor(out=dst[:, :, 0, :], in0=rt[0][:, :, :], in1=rt[1][:, :, :],
                                                       op=ALU.subtract), [rt[0].b, rt[1].b], [dst.b])
                S.op("dve", lambda e: e.scalar_tensor_tensor(out=rt[2][:, :, :], in0=pv[:, :, 0, :], scalar=r_ap,
                                                             in1=sinb, op0=ALU.mult, op1=ALU.mult),
                     [ps.b, cst_s.b, rb], [rt[2].b])
                S.op("dve", lambda e: e.scalar_tensor_tensor(out=rt[3][:, :, :], in0=pv[:, :, 1, :], scalar=r_ap,
                                                             in1=cosb, op0=ALU.mult, op1=ALU.mult),
                     [ps.b, cst_s.b, rb], [rt[3].b])
                S.op("dve", lambda e: e.tensor_tensor(out=dst[:, :, 1, :], in0=rt[2][:, :, :], in1=rt[3][:, :, :],
                                                       op=ALU.add), [rt[2].b, rt[3].b], [dst.b])

            def rms_rstd(ss_ap, out_ap, n, inv_n, reads, wbuf):
                S.op("dve", lambda e: e.tensor_scalar(out=out_ap, in0=ss_ap, scalar1=inv_n, scalar2=EPS,
                                                       op0=ALU.mult, op1=ALU.add), reads, [wbuf])
                S.op("pool", lambda e: e.tensor_tensor(out=out_ap, in0=out_ap, in1=nhalf[:, 0:n], op=ALU.pow),
                     [wbuf, nhalf.b], [wbuf])

            def front(ci, bs=None):
                full = ci >= NPREV
                last_prev = (ci == NPREV - 1)
                s = ci % 2
                x_s, c_s = xt[s], cst_t[s]
                hT = hT2[s]
                h_, krot_, rt_, v_, kdec_, stat_, th = bs if bs is not None else (h, krot, rt, v, kdec, stat, "f")

                def gate_pre(half):
                    ps_g = inproj(th, hT, O_RG + half * 512, 512)
                    sg = sgt[half]
                    S.op("act", lambda e, ps_g=ps_g, sg=sg: e.activation(out=sg[:, :], in_=ps_g[:, :], func=AF.Tanh,
                                                                         scale=rr2[s][:, 1:2]), [ps_g.b, rr2[s].b], [sg.b])
                    S.op("dve", lambda e, ps_g=ps_g, sg=sg: e.scalar_tensor_tensor(
                        out=sg[:, :], in0=sg[:, :], scalar=1.0, in1=ps_g[:, :], op0=ALU.add, op1=ALU.mult),
                         [ps_g.b, sg.b], [sg.b])
                rr = rr2[s]
                r_ap = rr[:, 0:1]
                rh_ap = rr[:, 1:2]
                S.op("act", lambda e: e.activation(out=h_[:, :], in_=x_s[:, :], func=AF.Square,
                                                   accum_out=stat_[:, 0:1]), [x_s.b], [h_.b, stat_.b])
                rms_rstd(stat_[:, 0:1], rr[:, 0:1], 1, 1.0 / D, [stat_.b], rr.b)
                S.op("dve", lambda e: e.tensor_scalar(out=rr[:, 1:2], in0=rr[:, 0:1], scalar1=0.5, scalar2=None,
                                                      op0=ALU.mult), [rr.b], [rr.b])
                ps_xa, ps_xb = psum(th), psum(th)
                for k in range(8):
                    pp = ps_xa if k < 4 else ps_xb
                    S.op("pe", lambda e, k=k, pp=pp: e.transpose(pp[:, (k % 4) * 128:(k % 4 + 1) * 128],
                                                                 x_s[:, k * 128:(k + 1) * 128],
                                                                 C[:, C_IDENT:C_IDENT + 128]),
                         [x_s.b, C.b], [pp.b], signal=(k % 4 == 3))
                S.op("act", lambda e: e.activation(out=hT[:, 0:4, :].rearrange("p k t -> p (k t)"), in_=ps_xa[:, :],
                                                   func=AF.Copy), [ps_xa.b], [hT.b])
                S.op("dve", lambda e: e.tensor_copy(out=hT[:, 4:8, :].rearrange("p k t -> p (k t)"), in_=ps_xb[:, :]),
                     [ps_xb.b], [hT.b])
                yield th
                ps_k = inproj(th, hT, O_RK, 512)
                rotary(ps_k, c_s, krot_, rt_, r_ap, rr.b)
                yield th
                ps_v1 = inproj(th, hT, O_RV, 512)
                S.op("act", lambda e: e.activation(out=v_[:, 0:512], in_=ps_v1[:, :], func=AF.Copy, scale=r_ap),
                     [ps_v1.b, rr.b], [v_.b])
                yield th
                ps_v2 = inproj(th, hT, O_RV + 512, 512)
                S.op("act", lambda e: e.activation(out=v_[:, 512:1024], in_=ps_v2[:, :], func=AF.Copy, scale=r_ap),
                     [ps_v2.b, rr.b], [v_.b])
                yield th
                if full:
                    ps_q = inproj(th, hT, O_RQ, 512)
                    rotary(ps_q, c_s, qrot, rt_, r_ap, rr.b)
                S.op("dve", lambda e: e.tensor_tensor(
                    out=kdec_[:, :, :], in0=krot_[:, :, :, :].rearrange("p h t d -> p h (t d)"),
                    in1=C[:, C_KDEC:C_KDEC + 4].unsqueeze(2).broadcast_to([128, 4, 128]), op=ALU.mult),
                     [krot_.b, C.b], [kdec_.b])
                yield th
                if full:
                    gate_pre(0)
                    yield th
                    ps_t = psum(th)
                    ptv = ps_t[:, :].bitcast(BF16)
                    for hh in range(4):
                        S.op("pe", lambda e, hh=hh: e.transpose(ptv[:, hh * 128:(hh + 1) * 128],
                                                                qrot[:, hh, :, :].rearrange("p t d -> p (t d)"),
                                                                identb[:, :]), [qrot.b, CB.b], [ps_t.b], signal=False)
                    for hh in range(4):
                        S.op("pe", lambda e, hh=hh: e.transpose(ptv[:, 512 + hh * 128:512 + (hh + 1) * 128],
                                                                krot_[:, hh, :, :].rearrange("p t d -> p (t d)"),
                                                                identb[:, :]), [krot_.b, CB.b], [ps_t.b], signal=(hh == 3))
                    S.op("dve", lambda e: e.tensor_tensor(out=qT[:, :, :].rearrange("p h t -> p (h t)"),
                                                          in0=ptv[:, 0:512], in1=C[:, C_DQROW:C_DQROW + 512],
                                                          op=ALU.mult), [ps_t.b, C.b], [qT.b])
                    S.op("dve", lambda e: e.tensor_copy(out=kT[:, :, :].rearrange("p h t -> p (h t)"),
                                                        in_=ptv[:, 512:1024]), [ps_t.b], [kT.b])
                    yield th
                    ps_s = psum(th)
                    for hh in range(4):
                        S.op("pe", lambda e, hh=hh: e.matmul(ps_s[:, hh * 128:(hh + 1) * 128], lhsT=kT[:, hh, :],
                                                             rhs=qT[:, hh, :], start=True, stop=True),
                             [kT.b, qT.b], [ps_s.b], signal=(hh == 3))
                    S.op("dve", lambda e: e.tensor_tensor(out=SdT[:, :, :].rearrange("p h t -> p (h t)"),
                                                          in0=ps_s[:, :], in1=C[:, C_DTP:C_DTP + 512], op=ALU.mult),
                         [ps_s.b, C.b], [SdT.b])
                    yield th
                    ps_o = [psum(th), psum(th)]
                    for hh in range(4):
                        po = ps_o[hh // 2]
                        c0 = (hh % 2) * 256
                        S.op("pe", lambda e, hh=hh, po=po, c0=c0: e.matmul(
                            po[:, c0:c0 + 256], lhsT=SdT[:, hh, :], rhs=v_[:, hh * 256:(hh + 1) * 256],
                            start=True, stop=False), [SdT.b, v_.b], [po.b], signal=False)
                        S.op("pe", lambda e, hh=hh, po=po, c0=c0: e.matmul(
                            po[:, c0:c0 + 256], lhsT=qT[:, hh, :], rhs=Rb[:, hh, :],
                            start=False, stop=True), [qT.b, Rb.b], [po.b], signal=(hh % 2 == 1))
                    for hh in range(4):
                        po = ps_o[hh // 2]
                        c0 = (hh % 2) * 256
                        S.op("dve", lambda e, hh=hh, po=po, c0=c0: e.bn_stats(out=stat_[:, 32 + hh * 6:38 + hh * 6],
                                                                              in_=po[:, c0:c0 + 256]),
                             [po.b], [stat_.b])
                        S.op("dve", lambda e, hh=hh: e.bn_aggr(out=stat_[:, 56 + hh * 2:58 + hh * 2],
                                                               in_=stat_[:, 32 + hh * 6:38 + hh * 6]), [stat_.b], [stat_.b])
                    var_ap = stat_[:, 56:64].rearrange("p (h t) -> p h t", t=2)[:, :, 1]
                    mean_ap = stat_[:, 56:64].rearrange("p (h t) -> p h t", t=2)[:, :, 0]
                    S.op("dve", lambda e: e.tensor_scalar(out=stat_[:, 2:6], in0=var_ap, scalar1=EPS, scalar2=None,
                                                           op0=ALU.add), [stat_.b], [stat_.b])
                    S.op("pool", lambda e: e.tensor_tensor(out=stat_[:, 2:6], in0=stat_[:, 2:6], in1=nhalf[:, 0:4],
                                                           op=ALU.pow), [stat_.b, nhalf.b], [stat_.b])
                    S.op("dve", lambda e: e.tensor_scalar(out=stat_[:, 2:6], in0=stat_[:, 2:6], scalar1=r_ap, scalar2=None,
                                                          op0=ALU.mult), [stat_.b, rr.b], [stat_.b])
                    S.op("dve", lambda e: e.scalar_tensor_tensor(out=stat_[:, 28:32], in0=mean_ap, scalar=-1.0,
                                                                 in1=stat_[:, 2:6], op0=ALU.mult, op1=ALU.mult),
                         [stat_.b], [stat_.b])
                    for hh in range(4):
                        po = ps_o[hh // 2]
                        c0 = (hh % 2) * 256
                        S.op("act", lambda e, hh=hh, po=po, c0=c0: e.activation(
                            out=gnt[:, hh * 256:(hh + 1) * 256], in_=po[:, c0:c0 + 256], func=AF.Identity,
                            bias=stat_[:, 28 + hh:29 + hh], scale=stat_[:, 2 + hh:3 + hh]), [po.b, stat_.b], [gnt.b])
                yield th
                ps_kv = [psum(th), psum(th)]
                for hh in range(4):
                    pk = ps_kv[hh // 2]
                    c0 = (hh % 2) * 256
                    S.op("pe", lambda e, hh=hh, pk=pk, c0=c0: e.matmul(
                        pk[:, c0:c0 + 256], lhsT=kdec_[:, hh, :], rhs=v_[:, hh * 256:(hh + 1) * 256],
                        start=True, stop=True), [kdec_.b, v_.b], [pk.b], signal=(hh % 2 == 1))
                for hh in range(4):
                    pk = ps_kv[hh // 2]
                    c0 = (hh % 2) * 256
                    S.op("dve", lambda e, hh=hh, pk=pk, c0=c0: e.scalar_tensor_tensor(
                        out=R[:, hh, :], in0=R[:, hh, :], scalar=cdec[hh], in1=pk[:, c0:c0 + 256],
                        op0=ALU.mult, op1=ALU.add), [pk.b, R.b], [R.b])
                S.op("act", lambda e: e.activation(out=Rb[:, :, :], in_=R[:, :, :], func=AF.Copy), [R.b], [Rb.b])
                yield th
                if full or last_prev:
                    rs = ci % 3
                    ps_kv2 = inproj(th, hT, O_SK, 256)
                    S.op("act", lambda e: e.activation(out=sqk[:, :], in_=ps_kv2[:, 0:128], func=AF.Square, scale=r_ap),
                         [ps_kv2.b, rr.b], [sqk.b])
                    S.op("act", lambda e: e.activation(out=vsr[rs][:, :, 0:64],
                                                       in_=ps_kv2[:, 128:256].rearrange("p (j d) -> p j d", j=2),
                                                       func=AF.Copy, scale=r_ap), [ps_kv2.b, rr.b], [vsr[rs].b])
                    S.op("dve", lambda e: e.tensor_reduce(out=stat_[:, 16:18],
                                                          in_=sqk[:, :].rearrange("p (h d) -> p h d", h=2),
                                                          axis=AX.X, op=ALU.add), [sqk.b], [stat_.b])
                    rms_rstd(stat_[:, 16:18], stat_[:, 18:20], 2, 1.0 / 64, [stat_.b], stat_.b)
                    S.op("dve", lambda e: e.tensor_scalar(out=stat_[:, 18:20], in0=stat_[:, 18:20], scalar1=r_ap,
                                                          scalar2=None, op0=ALU.mult), [stat_.b, rr.b], [stat_.b])
                    for hk in range(2):
                        S.op("dve", lambda e, hk=hk: e.scalar_tensor_tensor(
                            out=kn[:, hk * 64:(hk + 1) * 64], in0=ps_kv2[:, hk * 64:(hk + 1) * 64],
                            scalar=stat_[:, 18 + hk:19 + hk], in1=gqk[:, :], op0=ALU.mult, op1=ALU.mult),
                             [ps_kv2.b, stat_.b, gqk.b], [kn.b])
                    yield th
                    if full:
                        gate_pre(1)
                        yield th
                    ps_t2 = psum(th)
                    ptv2 = ps_t2[:, :].bitcast(BF16)
                    S.op("pe", lambda e: e.transpose(ptv2[:, 0:128], kn[:, :], identb[:, :]), [kn.b, CB.b], [ps_t2.b])
                    S.op("act", lambda e: e.activation(out=kTr[rs][:, :], in_=ptv2[:, 0:128], func=AF.Copy),
                         [ps_t2.b], [kTr[rs].b])
                if not full:
                    return
                ret = ret2[s]
                for half in range(2):
                    sg = sgt[half]
                    S.op("dve", lambda e, half=half, sg=sg: e.tensor_tensor(
                        out=ret[:, half * 512:(half + 1) * 512], in0=gnt[:, half * 512:(half + 1) * 512], in1=sg[:, :],
                        op=ALU.mult), [gnt.b, sg.b], [ret.b])
                    yield th

            def back(ci):
                j_own = ci - NPREV
                s = ci % 2
                x_s = xt[s]
                hT, ret, oTn = hT2[s], ret2[s], oTn2[s]
                def gate_pre(idx, col0):
                    ps_g = inproj("b", hT, col0, 512)
                    S.op("act", lambda e, ps_g=ps_g, idx=idx: e.activation(out=sgb[idx][:, :], in_=ps_g[:, :],
                                                                           func=AF.Tanh, scale=rr2[s][:, 1:2]),
                         [ps_g.b, rr2[s].b], [sgb[idx].b])

                rs = ci % 3
                rp = (ci - 1) % 3
                ps_sq = inproj("b", hT, O_SQ, 512)
                rr = rr2[s]
                S.op("act", lambda e: e.activation(out=sqq[:, :], in_=ps_sq[:, :], func=AF.Square, scale=rr[:, 0:1]),
                     [ps_sq.b, rr.b], [sqq.b])
                S.op("dve", lambda e: e.tensor_reduce(out=statb[:, 8:16],
                                                      in_=sqq[:, :].rearrange("p (h d) -> p h d", h=8),
                                                      axis=AX.X, op=ALU.add), [sqq.b], [statb.b])
                rms_rstd(statb[:, 8:16], statb[:, 20:28], 8, 1.0 / 64, [statb.b], statb.b)
                S.op("dve", lambda e: e.tensor_scalar(out=statb[:, 20:28], in0=statb[:, 20:28], scalar1=rr[:, 0:1],
                                                      scalar2=None, op0=ALU.mult), [statb.b, rr.b], [statb.b])
                S.op("dve", lambda e: e.tensor_tensor(
                    out=qn[:, :, :, :].rearrange("p i j d -> p j i d"),
                    in0=ps_sq[:, :].rearrange("p (j i d) -> p j i d", j=2, i=4),
                    in1=statb[:, 20:28].rearrange("p (j i) -> p j i", j=2).unsqueeze(3).broadcast_to([128, 2, 4, 64]),
                    op=ALU.mult), [ps_sq.b, statb.b], [qn.b])
                yield "b"
                gate_pre(0, O_GA)
                yield "b"
                gate_pre(1, O_GA + 512)
                yield "b"
                ps_t3 = psum("b")
                ptv3 = ps_t3[:, :].bitcast(BF16)
                for i in range(4):
                    S.op("pe", lambda e, i=i: e.transpose(ptv3[:, i * 128:(i + 1) * 128],
                                                          qn[:, i, :, :].rearrange("p j d -> p (j d)"),
                                                          identb[:, :]), [qn.b, CB.b], [ps_t3.b], signal=(i == 3))
                S.op("act", lambda e: e.activation(out=qnT[:, :, :].rearrange("p i t -> p (i t)"),
                                                   in_=ptv3[:, 0:512], func=AF.Copy), [ps_t3.b], [qnT.b])
                yield "b"
                first_own = (ci == NPREV)
                ebs = {}
                for jg in range(2):
                    for ch, (ring, msk) in enumerate(((rp, maskP0 if first_own else maskP), (rs, maskC))):
                        ps_sc = psum("b")
                        S.op("pe", lambda e, jg=jg, ring=ring, ps_sc=ps_sc: e.matmul(
                            ps_sc[:, :], lhsT=kTr[ring][jg * 64:(jg + 1) * 64, :],
                            rhs=qnT[jg * 64:(jg + 1) * 64, :, :].rearrange("p i t -> p (i t)"),
                            start=True, stop=True), [kTr[ring].b, qnT.b], [ps_sc.b])
                        ebuf = eb[eb_i[0] % 4]
                        eb_i[0] += 1
                        ebs[(jg, ch)] = ebuf
                        S.op("act", lambda e, ps_sc=ps_sc, ebuf=ebuf: e.activation(
                            out=ebuf[:, :], in_=ps_sc[:, :], func=AF.Exp, bias=negc[:, 0:1], scale=1.0),
                             [ps_sc.b, negc.b], [ebuf.b])
                        S.op("dve", lambda e, ebuf=ebuf, msk=msk: e.tensor_tensor(
                            out=ebuf[:, :].rearrange("p (i t) -> p i t", i=4),
                            in0=ebuf[:, :].rearrange("p (i t) -> p i t", i=4),
                            in1=msk[:, :].unsqueeze(1).broadcast_to([128, 4, 128]), op=ALU.mult),
                             [ebuf.b, msk.b], [ebuf.b])
                        yield "b"
                gate_pre(2, O_GB)
                yield "b"
                ps_pv = [psum("b"), psum("b")]
                for jg in range(2):
                    for i in range(4):
                        for ch, ring in enumerate((rp, rs)):
                            S.op("pe", lambda e, jg=jg, i=i, ch=ch, ring=ring: e.matmul(
                                ps_pv[jg][:, i * 65:(i + 1) * 65], lhsT=ebs[(jg, ch)][:, i * 128:(i + 1) * 128],
                                rhs=vsr[ring][:, jg, :], start=(ch == 0), stop=(ch == 1)),
                                 [vsr[ring].b, ebs[(jg, ch)].b], [ps_pv[jg].b], signal=(i == 3 and ch == 1))
                for jg in range(2):
                    pvw = ps_pv[jg][:, 0:260].rearrange("p (i c) -> p i c", i=4)
                    S.op("dve", lambda e, jg=jg, pvw=pvw: e.tensor_tensor(
                        out=statb[:, 32 + jg * 4:36 + jg * 4].unsqueeze(2), in0=pvw[:, :, 64:65],
                        in1=sinkexp[:, jg * 4:(jg + 1) * 4].unsqueeze(2), op=ALU.add),
                         [ps_pv[jg].b, sinkexp.b], [statb.b])
                    S.op("dve", lambda e, jg=jg: e.reciprocal(out=statb[:, 40 + jg * 4:44 + jg * 4],
                                                              in_=statb[:, 32 + jg * 4:36 + jg * 4]), [statb.b], [statb.b])
                    S.op("dve", lambda e, jg=jg, pvw=pvw: e.tensor_tensor(
                        out=on[:, jg * 4:(jg + 1) * 4, :], in0=pvw[:, :, 0:64],
                        in1=statb[:, 40 + jg * 4:44 + jg * 4].unsqueeze(2).broadcast_to([128, 4, 64]), op=ALU.mult),
                         [ps_pv[jg].b, statb.b], [on.b])
                yield "b"
                gate_pre(3, O_GB + 512)
                yield "b"
                ps = psum("b")
                transposes(lambda k: on[:, 2 * k:2 * k + 2, :].rearrange("p a d -> p (a d)"), 4, ps, [on.b])
                S.op("act", lambda e, ps=ps: e.activation(out=oTn[:, :, :].rearrange("p k t -> p (k t)"),
                                                          in_=ps[:, :].bitcast(BF16)[:, 0:512], func=AF.Copy),
                     [ps.b], [oTn.b])
                yield "b"
                ps = psum("b")
                transposes(lambda k: ret[:, k * 128:(k + 1) * 128], 8, ps, [ret.b])
                S.op("act", lambda e, ps=ps: e.activation(out=xT2[:, :, :].rearrange("p k t -> p (k t)"),
                                                          in_=ps[:, :].bitcast(BF16), func=AF.Copy), [ps.b], [xT2.b])
                yield "b"
                for half in range(2):
                    ps_y = psum("b")
                    for k in range(8):
                        S.op("pe", lambda e, k=k, ps_y=ps_y, half=half: e.matmul(
                            ps_y[:, :], lhsT=xT2[:, k, :], rhs=Wro[:, k, half * 512:(half + 1) * 512],
                            start=(k == 0), stop=(k == 7)), [xT2.b, Wro.b], [ps_y.b], signal=(k == 7))
                    sg = sgb[half]
                    S.op("dve", lambda e, ps_y=ps_y, sg=sg, half=half: e.scalar_tensor_tensor(
                        out=ma[:, half * 512:(half + 1) * 512], in0=sg[:, :], scalar=1.0, in1=ps_y[:, :],
                        op0=ALU.add, op1=ALU.mult), [ps_y.b, sg.b], [ma.b])
                    yield "b"
                for half in range(2):
                    ps_y = psum("b")
                    for k in range(4):
                        S.op("pe", lambda e, k=k, ps_y=ps_y, half=half: e.matmul(
                            ps_y[:, :], lhsT=oTn[:, k, :], rhs=Wso[:, k, half * 512:(half + 1) * 512],
                            start=(k == 0), stop=(k == 3)), [oTn.b, Wso.b], [ps_y.b], signal=(k == 3))
                    sg = sgb[2 + half]
                    S.op("dve", lambda e, ps_y=ps_y, sg=sg, half=half: e.scalar_tensor_tensor(
                        out=tb[:, half * 512:(half + 1) * 512], in0=sg[:, :], scalar=1.0, in1=ps_y[:, :],
                        op0=ALU.add, op1=ALU.mult), [ps_y.b, sg.b], [tb.b])
                    yield "b"
                S.op("dve", lambda e: e.tensor_tensor(out=tb[:, :], in0=tb[:, :], in1=ma[:, :], op=ALU.add),
                     [tb.b, ma.b], [tb.b])
                yield "b"
                ps = psum("b")
                transposes(lambda k: tb[:, k * 128:(k + 1) * 128], 8, ps, [tb.b])
                S.op("act", lambda e, ps=ps: e.activation(out=xT2[:, :, :].rearrange("p k t -> p (k t)"),
                                                          in_=ps[:, :].bitcast(BF16), func=AF.Copy), [ps.b], [xT2.b])
                yield "b"
                for half in range(2):
                    ps_y = psum("b")
                    for k in range(8):
                        S.op("pe", lambda e, k=k, ps_y=ps_y, half=half: e.matmul(
                            ps_y[:, :], lhsT=xT2[:, k, :], rhs=Wout[:, k, half * 512:(half + 1) * 512],
                            start=(k == 0), stop=(k == 7)), [xT2.b, Wout.b], [ps_y.b], signal=(k == 7))
                    S.op("dve", lambda e, ps_y=ps_y, half=half: e.tensor_tensor(
                        out=x_s[:, half * 512:(half + 1) * 512], in0=ps_y[:, :],
                        in1=x_s[:, half * 512:(half + 1) * 512], op=ALU.add), [ps_y.b, x_s.b], [x_s.b])
                    yield "b"
                S.dma("sp", st_x1[s], lambda e: e.dma_start(out=x1buf[j_own * 128:(j_own + 1) * 128, :], in_=x_s[:, :]),
                      reads=[x_s.b], writes=[x1_tiles[j_own]])
                S.op("act", lambda e: e.activation(out=h2[:, :], in_=x_s[:, :], func=AF.Square,
                                                   accum_out=statb[:, 0:1]), [x_s.b], [h2.b, statb.b])
                S.op("dve", lambda e: e.tensor_scalar(out=statb[:, 1:2], in0=statb[:, 0:1], scalar1=1.0 / D,
                                                       scalar2=EPS, op0=ALU.mult, op1=ALU.add), [statb.b], [statb.b])
                S.op("pool", lambda e: e.tensor_tensor(out=statb[:, 1:2], in0=statb[:, 1:2], in1=nhalf[:, 0:1],
                                                       op=ALU.pow), [statb.b, nhalf.b], [statb.b])
                S.op("dve", lambda e: e.tensor_scalar(out=h2[:, :], in0=x_s[:, :], scalar1=statb[:, 1:2],
                                                      scalar2=None, op0=ALU.mult), [x_s.b, statb.b], [h2.b])
                S.dma("sp", st_h2, lambda e: e.dma_start(out=h2buf[j_own * 128:(j_own + 1) * 128, :], in_=h2[:, :]),
                      reads=[h2.b], writes=[h2_tiles[j_own]])
                yield "b"
                ps_l = psum("b")
                for hf in range(2):
                    ps_a = psum("b")
                    for k4 in range(4):
                        k = hf * 4 + k4
                        S.op("pe", lambda e, k=k, k4=k4, ps_a=ps_a: e.transpose(
                            ps_a[:, k4 * 128:(k4 + 1) * 128], x_s[:, k * 128:(k + 1) * 128],
                            C[:, C_IDENT:C_IDENT + 128]), [x_s.b, C.b], [ps_a.b], signal=(k4 == 3))
                    S.op("act", lambda e, ps_a=ps_a: e.activation(out=x1T[:, :, :].rearrange("p k t -> p (k t)"),
                                                                  in_=ps_a[:, :], func=AF.Copy), [ps_a.b], [x1T.b])
                    for k4 in range(4):
                        k = hf * 4 + k4
                        S.op("pe", lambda e, k=k, k4=k4: e.matmul(ps_l[:, 0:36], lhsT=x1T[:, k4, :], rhs=wrs[:, k, :],
                                                                  start=(k == 0), stop=(k == 7)), [x1T.b, wrs.b],
                             [ps_l.b], signal=(k4 == 3))
                S.op("dve", lambda e: e.scalar_tensor_tensor(out=L[:, j_own, :], in0=ps_l[:, 0:36],
                                                             scalar=statb[:, 1:2], in1=C[:, C_BR:C_BR + 36],
                                                             op0=ALU.mult, op1=ALU.add), [ps_l.b, statb.b, C.b], [L.b])
                yield "b"

            def step(g, last):
                try:
                    last[0] = next(g)
                    release(last[0])
                    return True
                except StopIteration:
                    if last[0] is not None:
                        release(last[0])
                    return False

            def run_all(g):
                last = [None]
                while step(g, last):
                    pass

            setB = (ma, TV(xT2.t[:, 0:4, :].rearrange("p h (t d) -> p h t d", t=2), xT2.b),
                    [TV(eb[i].t[:, 0:256].rearrange("p (h d) -> p h d", h=4), eb[i].b) for i in range(4)],
                    tb, TV(xT2.t[:, 4:8, :], xT2.b), statp, "b")

            def interleave(gens):
                gens = [(g, [None]) for g in gens]
                while gens:
                    for item in list(gens):
                        if not step(item[0], item[1]):
                            gens.remove(item)

            load_tile(0)
            if NT > 1:
                load_tile(1)
            ci = 0
            folded = False
            zfilled = False
            while ci < NPREV:
                if wq:
                    issue_weights(len(wq) if ci >= NPREV - 2 else 3)
                if ci >= 4 and not zfilled:
                    zero_fill()
                    zfilled = True
                if not wq and ci < NPREV - 1 and not folded:
                    fold_rest()
                    folded = True
                if ci + 1 < NPREV:
                    interleave([front(ci), front(ci + 1, setB)])
                    done = 2
                else:
                    interleave([front(ci)])
                    done = 1
                for c2 in range(ci + 2, ci + 2 + done):
                    if c2 < NT:
                        load_tile(c2)
                ci += done
            if not zfilled:
                zero_fill()
            if wq:
                issue_weights(len(wq))
            if not folded:
                fold_rest()
            if NPREV % 2 == 1 and NPREV + 1 < NT:
                pass
            interleave([front(NPREV)])
            step_counts = {}

            def interleave_prop(named):
                live = {n: (g, [None]) for n, g in named}
                done = {n: 0 for n, _ in named}
                while live:
                    n = min(live, key=lambda k: (done[k] + 1) / float(step_counts.get(k, 1e9)))
                    if ORDER is not None and len(live) == 2:
                        pos = done["front"] + done["back"]
                        if pos < len(ORDER):
                            n = "front" if ORDER[pos] == "f" else "back"
                    if step(live[n][0], live[n][1]):
                        done[n] += 1
                    else:
                        del live[n]
                LAST_COUNTS.update(step_counts)

            for ci in range(NPREV, NT):
                named = []
                if ci + 1 < NT:
                    named.append(("front", front(ci + 1)))
                named.append(("back", back(ci)))
                if len(named) == 2 and "front" not in step_counts:
                    live = {n: (g, [None]) for n, g in named}
                    cnt = {n: 0 for n in live}
                    while live:
                        for n in list(live):
                            if step(live[n][0], live[n][1]):
                                cnt[n] += 1
                            else:
                                del live[n]
                    step_counts.update(cnt)
                elif len(named) == 2:
                    interleave_prop(named)
                else:
                    interleave([g for _, g in named])
                if ci + 2 < NT:
                    load_tile(ci + 2)
            S.barrier()

        if not moe:
            with contextlib.ExitStack() as p2:
                tt = [T(p2.enter_context(nc.sbuf_tensor("cp%d" % i, [128, D], F32)), "cp%d" % i) for i in range(2)]
                st_l = [S.stream("cpl%d" % i) for i in range(2)]
                st_o = S.stream("cpo")
                for j in range(NOWN):
                    t = tt[j % 2]
                    S.dma("sp", st_l[j % 2], lambda e, j=j, t=t: e.dma_start(out=t[:, :], in_=x1buf[j * 128:(j + 1) * 128, :]),
                          writes=[t.b])
                    S.dma("sp", st_o, lambda e, j=j, t=t: e.dma_start(out=y[j * 128:(j + 1) * 128, :], in_=t[:, :]),
                          reads=[t.b])
                S.barrier()
            S.emit()
            return nc

        NJ = NOWN
        with contextlib.ExitStack() as p2:
            def sb2(name, shape, dt):
                return T(p2.enter_context(nc.sbuf_tensor("sb_" + name, list(shape), dt)), name)

            pass
        p23 = stack
        wg = [sb("wg%d" % i, [128, 8, DFF], BF16) for i in range(2)]
        wu = [sb("wu%d" % i, [128, 8, DFF], BF16) for i in range(2)]
        wd = [sb("wd%d" % i, [128, 4, D], BF16) for i in range(2)]
        st_we = [S.stream("we%d" % i) for i in range(2)]

        def load_w(ex):
            s = ex % 2
            S.dma("pool", st_we[s], lambda e: e.dma_start(out=wg[s][:, :, :],
                                                           in_=w_gate[ex].rearrange("(k p) n -> p k n", p=128)),
                  writes=[wg[s].b])
            S.dma("pool", st_we[s], lambda e: e.dma_start(out=wu[s][:, :, :],
                                                           in_=w_up[ex].rearrange("(k p) n -> p k n", p=128)),
                  writes=[wu[s].b])
            S.dma("pool", st_we[s], lambda e: e.dma_start(out=wd[s][:, :, :],
                                                           in_=w_down[ex].rearrange("(k p) n -> p k n", p=128)),
                  writes=[wd[s].b])
            S.seal(st_we[s], [wg[s].b, wu[s].b, wd[s].b])


        load_w(0)
        load_w(1)
        regcache = {}

        def bc_reg(e):
            if "bc" not in regcache:
                regcache["bc"] = e.to_reg(NSLOT - 1)
            return regcache["bc"]

        slot_i = sb("slot_i", [128, 2, NJ], I32)
        wts = sb("wts", [128, 2, NJ], F32)
        with contextlib.ExitStack() as p2:
            def sb2(name, shape, dt):
                return T(p2.enter_context(nc.sbuf_tensor("sb_" + name, list(shape), dt)), name)

            gmax = sb2("gmax", [128, NJ], F32)
            ohg = sb2("ohg", [128, NJ, 4], F32)
            ge = sb2("ge", [128, NJ, 4], F32)
            gsum = sb2("gsum", [128, NJ], F32)
            gprob = sb2("gprob", [128, NJ], F32)
            elm = sb2("elm", [128, NJ, 4, 8], F32)
            sel = sb2("sel", [128, NJ, 8], F32)
            m1 = sb2("m1", [128, NJ], F32)
            m2 = sb2("m2", [128, NJ], F32)
            oh = [sb2("oh%d" % i, [128, NJ, 8], F32) for i in range(2)]
            sel2 = sb2("sel2", [128, NJ, 8], F32)
            Ek = [sb2("E%d" % i, [128, NJ, 4, 8], F32) for i in range(2)]
            M = sb2("M", [128, NJ, 32], F32)
            pos = sb2("pos", [128, NJ, 32], F32)
            base = sb2("base", [128, NJ, 32], F32)
            tot = sb2("tot", [128, NJ, 32], F32)
            ones_f = sb2("ones_f", [128, 128], F32)
            tmp = sb2("tmp", [128, NJ, 32], F32)
            slot_f = sb2("slot_f", [128, 2, NJ], F32)
            dd = sb2("dd", [128, NJ], F32)

            V = "dve"
            Lg = L[:, :, 0:4]
            Le = L[:, :, 4:36]
            S.op(V, lambda e: e.memset(ones_f[:, :], 1.0), [], [ones_f.b])
            S.op(V, lambda e: e.tensor_reduce(out=gmax[:, :], in_=Lg, axis=AX.X, op=ALU.max), [L.b], [gmax.b])
            S.op(V, lambda e: e.tensor_tensor(out=ohg[:, :, :], in0=Lg,
                                              in1=gmax[:, :].unsqueeze(2).broadcast_to([128, NJ, 4]),
                                              op=ALU.is_equal), [L.b, gmax.b], [ohg.b])
            S.op(V, lambda e: e.tensor_tensor(out=ge[:, :, :], in0=Lg,
                                              in1=gmax[:, :].unsqueeze(2).broadcast_to([128, NJ, 4]),
                                              op=ALU.subtract), [L.b, gmax.b], [ge.b])
            S.op("act", lambda e: e.activation(out=ge[:, :, :], in_=ge[:, :, :], func=AF.Exp), [ge.b], [ge.b])
            S.op(V, lambda e: e.tensor_reduce(out=gsum[:, :], in_=ge[:, :, :], axis=AX.X, op=ALU.add), [ge.b], [gsum.b])
            S.op(V, lambda e: e.reciprocal(out=gprob[:, :], in_=gsum[:, :]), [gsum.b], [gprob.b])
            S.op(V, lambda e: e.tensor_tensor(out=elm[:, :, :, :], in0=Le.rearrange("p j (g x) -> p j g x", g=4),
                                              in1=ohg[:, :, :].unsqueeze(3).broadcast_to([128, NJ, 4, 8]),
                                              op=ALU.mult), [L.b, ohg.b], [elm.b])
            S.op(V, lambda e: e.tensor_reduce(out=sel[:, :, :], in_=elm[:, :, :, :].rearrange("p j g x -> p j x g"),
                                              axis=AX.X, op=ALU.add), [elm.b], [sel.b])
            S.op(V, lambda e: e.tensor_reduce(out=m1[:, :], in_=sel[:, :, :], axis=AX.X, op=ALU.max), [sel.b], [m1.b])
            S.op(V, lambda e: e.tensor_tensor(out=oh[0][:, :, :], in0=sel[:, :, :],
                                              in1=m1[:, :].unsqueeze(2).broadcast_to([128, NJ, 8]),
                                              op=ALU.is_equal), [sel.b, m1.b], [oh[0].b])
            S.op(V, lambda e: e.scalar_tensor_tensor(out=sel2[:, :, :], in0=oh[0][:, :, :], scalar=-1e30,
                                                     in1=sel[:, :, :], op0=ALU.mult, op1=ALU.add),
                 [oh[0].b, sel.b], [sel2.b])
            S.op(V, lambda e: e.tensor_reduce(out=m2[:, :], in_=sel2[:, :, :], axis=AX.X, op=ALU.max), [sel2.b], [m2.b])
            S.op(V, lambda e: e.tensor_tensor(out=oh[1][:, :, :], in0=sel2[:, :, :],
                                              in1=m2[:, :].unsqueeze(2).broadcast_to([128, NJ, 8]),
                                              op=ALU.is_equal), [sel2.b, m2.b], [oh[1].b])
            S.op(V, lambda e: e.tensor_tensor(out=dd[:, :], in0=m2[:, :], in1=m1[:, :], op=ALU.subtract),
                 [m1.b, m2.b], [dd.b])
            S.op("act", lambda e: e.activation(out=dd[:, :], in_=dd[:, :], func=AF.Exp), [dd.b], [dd.b])
            S.op(V, lambda e: e.tensor_scalar(out=dd[:, :], in0=dd[:, :], scalar1=1.0, scalar2=None, op0=ALU.add),
                 [dd.b], [dd.b])
            S.op(V, lambda e: e.reciprocal(out=dd[:, :], in_=dd[:, :]), [dd.b], [dd.b])
            S.op(V, lambda e: e.tensor_scalar(out=gprob[:, :], in0=gprob[:, :], scalar1=0.5, scalar2=None,
                                              op0=ALU.mult), [gprob.b], [gprob.b])
            S.op(V, lambda e: e.tensor_tensor(out=wts[:, 0, :], in0=dd[:, :], in1=gprob[:, :], op=ALU.mult),
                 [dd.b, gprob.b], [wts.b])
            S.op(V, lambda e: e.tensor_tensor(out=wts[:, 1, :], in0=gprob[:, :], in1=wts[:, 0, :], op=ALU.subtract),
                 [gprob.b, wts.b], [wts.b])
            for kk in range(2):
                S.op(V, lambda e, kk=kk: e.tensor_tensor(
                    out=Ek[kk][:, :, :, :], in0=ohg[:, :, :].unsqueeze(3).broadcast_to([128, NJ, 4, 8]),
                    in1=oh[kk][:, :, :].unsqueeze(2).broadcast_to([128, NJ, 4, 8]), op=ALU.mult),
                     [ohg.b, oh[kk].b], [Ek[kk].b])
            S.op(V, lambda e: e.tensor_tensor(out=M[:, :, :], in0=Ek[0][:, :, :, :].rearrange("p j g x -> p j (g x)"),
                                              in1=Ek[1][:, :, :, :].rearrange("p j g x -> p j (g x)"), op=ALU.add),
                 [Ek[0].b, Ek[1].b], [M.b])
            NCOL = NJ * 32
            Mf = M[:, :, :].rearrange("p j x -> p (j x)")
            for c0 in range(0, NCOL, 512):
                cw = min(512, NCOL - c0)
                ps1 = psum()
                S.op("pe", lambda e, c0=c0, cw=cw, ps1=ps1: e.matmul(ps1[:, 0:cw], lhsT=C[:, C_U:C_U + 128],
                                                                     rhs=Mf[:, c0:c0 + cw], start=True, stop=True),
                     [C.b, M.b], [ps1.b])
                S.op(V, lambda e, c0=c0, cw=cw, ps1=ps1: e.tensor_copy(
                    out=pos[:, :, :].rearrange("p j x -> p (j x)")[:, c0:c0 + cw], in_=ps1[:, 0:cw]),
                     [ps1.b], [pos.b])
                ps2 = psum()
                S.op("pe", lambda e, c0=c0, cw=cw, ps2=ps2: e.matmul(ps2[:, 0:cw], lhsT=ones_f[:, :],
                                                                     rhs=Mf[:, c0:c0 + cw], start=True, stop=True),
                     [ones_f.b, M.b], [ps2.b])
                S.op(V, lambda e, c0=c0, cw=cw, ps2=ps2: e.tensor_copy(
                    out=tot[:, :, :].rearrange("p j x -> p (j x)")[:, c0:c0 + cw], in_=ps2[:, 0:cw]),
                     [ps2.b], [tot.b])
            S.op(V, lambda e: e.tensor_copy(out=base[:, 0, :], in_=C[:, C_EC:C_EC + 32]), [C.b], [base.b])
            for j in range(1, NJ):
                S.op(V, lambda e, j=j: e.tensor_tensor(out=base[:, j, :], in0=base[:, j - 1, :], in1=tot[:, j - 1, :],
                                                       op=ALU.add), [base.b, tot.b], [base.b])
            S.op(V, lambda e: e.tensor_tensor(out=pos[:, :, :], in0=pos[:, :, :], in1=base[:, :, :], op=ALU.add),
                 [pos.b, base.b], [pos.b])
            S.op(V, lambda e: e.tensor_tensor(out=tmp[:, :, :], in0=pos[:, :, :],
                                              in1=C[:, C_EC:C_EC + 32].unsqueeze(1).broadcast_to([128, NJ, 32]),
                                              op=ALU.subtract), [pos.b, C.b], [tmp.b])
            S.op(V, lambda e: e.tensor_scalar(out=tmp[:, :, :], in0=tmp[:, :, :], scalar1=float(CAP) - 0.5,
                                              scalar2=1.0e6, op0=ALU.is_gt, op1=ALU.mult), [tmp.b], [tmp.b])
            S.op(V, lambda e: e.tensor_tensor(out=pos[:, :, :], in0=pos[:, :, :], in1=tmp[:, :, :], op=ALU.add),
                 [pos.b, tmp.b], [pos.b])
            for kk in range(2):
                S.op(V, lambda e, kk=kk: e.tensor_tensor(out=tmp[:, :, :],
                                                         in0=Ek[kk][:, :, :, :].rearrange("p j g x -> p j (g x)"),
                                                         in1=pos[:, :, :], op=ALU.mult), [Ek[kk].b, pos.b], [tmp.b])
                S.op(V, lambda e, kk=kk: e.tensor_reduce(out=slot_f[:, kk, :], in_=tmp[:, :, :], axis=AX.X,
                                                         op=ALU.add), [tmp.b], [slot_f.b])
            S.op(V, lambda e: e.tensor_copy(out=slot_i[:, :, :], in_=slot_f[:, :, :]), [slot_f.b], [slot_i.b])

            for b_ in h2_tiles:
                b_.w = Tok(st_h2.sem, st_h2.count)
            gfb = sb2("gfb", [128, D], F32)
            st_gf = S.stream("gffn")
            S.dma("sp", st_gf, lambda e: e.dma_start(out=gfb[:, :], in_=gffn.broadcast_to([128, D])), writes=[gfb.b])
            NHB = 16
            hb = [sb2("hb%d" % i, [128, D], BF16) for i in range(NHB)]
            st_hb = [S.stream("hb%d" % i) for i in range(NHB)]
            st_sc = [S.stream("scat%d" % i) for i in range(NHB)]
            xs_all = Buf("xs_all")
            for j in range(NJ):
                t = hb[j % NHB]
                S.dma("sp", st_hb[j % NHB], lambda e, j=j, t=t: e.dma_start(out=t[:, :], in_=h2buf[j * 128:(j + 1) * 128, :]),
                      reads=[h2_tiles[j]], writes=[t.b])
                S.op("dve", lambda e, t=t: e.tensor_tensor(
                    out=t[:, :], in0=t[:, :], in1=gfb[:, :], op=ALU.mult), [t.b, gfb.b], [t.b])
                for kk in range(2):
                    S.dma("pool", st_sc[j % NHB], lambda e, j=j, kk=kk, t=t: e.indirect_dma_start(
                        out=xsbuf[0:NSLOT, :], out_offset=bass.IndirectOffsetOnAxis(ap=slot_i[:, kk, j:j + 1], axis=0),
                        in_=t[:, :], in_offset=None, bounds_check=bc_reg(e), oob_is_err=False),
                          reads=[t.b, slot_i.b], writes=[])
            S.barrier()

        NB = CAP // 128
        st_ys = [S.stream("ys%d" % i) for i in range(2)]
        with contextlib.ExitStack() as p3:
            def sb3(name, shape, dt):
                return T(p3.enter_context(nc.sbuf_tensor("sb_" + name, list(shape), dt)), name)

            xb = [sb3("xb%d" % i, [128, D], BF16) for i in range(3)]
            st_xb = [S.stream("xb%d" % i) for i in range(3)]
            xT = [sb3("xTe%d" % i, [128, 8, CAP], BF16) for i in range(2)]
            gs = [sb3("gs%d" % i, [128, CAP], BF16) for i in range(2)]
            aT = [sb3("aT%d" % i, [128, 4, CAP], BF16) for i in range(2)]
            yt = [sb3("yt%d" % i, [128, D], BF16) for i in range(2)]
            xb_i = [0]
            gs_i = [0]
            yt_i = [0]

            def load_x(ex):
                res = []
                for bi in range(NB):
                    t = xb[xb_i[0] % 3]
                    stx = st_xb[xb_i[0] % 3]
                    xb_i[0] += 1
                    r0 = ex * CAP + bi * 128
                    S.dma("sp", stx, lambda e, t=t, r0=r0: e.dma_start(out=t[:, :], in_=xsbuf[r0:r0 + 128, :]),
                          writes=[t.b])
                    res.append(t)
                return res

            def do_transposes(ex):
                xbl = load_x(ex)
                xTe = xT[ex % 2]
                for bi in range(NB):
                    ps = psum()
                    t = xbl[bi]
                    transposes(lambda k, t=t: t[:, k * 128:(k + 1) * 128], 8, ps, [t.b])
                    pv = ps[:, :].bitcast(BF16).rearrange("p (k t) -> p k t", k=8)
                    if bi % 2 == 0:
                        S.op("act", lambda e, pv=pv, bi=bi, xTe=xTe: e.activation(
                            out=xTe[:, :, bi * 128:(bi + 1) * 128], in_=pv, func=AF.Copy), [ps.b, t.b], [xTe.b])
                    else:
                        S.op("dve", lambda e, pv=pv, bi=bi, xTe=xTe: e.tensor_copy(
                            out=xTe[:, :, bi * 128:(bi + 1) * 128], in_=pv), [ps.b, t.b], [xTe.b])

            def gate_up(ex):
                s = ex % 2
                xTe = xT[s]
                aTe = aT[s]
                for f in range(4):
                    ps_g = psum()
                    for k in range(8):
                        S.op("pe", lambda e, k=k, f=f, ps_g=ps_g: e.matmul(
                            ps_g[:, 0:CAP], lhsT=wg[s][:, k, f * 128:(f + 1) * 128], rhs=xTe[:, k, :],
                            start=(k == 0), stop=(k == 7)), [wg[s].b, xTe.b], [ps_g.b], signal=(k == 7))
                    ps_u = psum()
                    for k in range(8):
                        S.op("pe", lambda e, k=k, f=f, ps_u=ps_u: e.matmul(
                            ps_u[:, 0:CAP], lhsT=wu[s][:, k, f * 128:(f + 1) * 128], rhs=xTe[:, k, :],
                            start=(k == 0), stop=(k == 7)), [wu[s].b, xTe.b], [ps_u.b], signal=(k == 7))
                    g_ = gs[gs_i[0] % 2]
                    gs_i[0] += 1
                    S.op("act", lambda e, ps_g=ps_g, g_=g_: e.activation(out=g_[:, :], in_=ps_g[:, 0:CAP], func=AF.Tanh,
                                                                         scale=0.5), [ps_g.b], [g_.b])
                    S.op("dve", lambda e, ps_g=ps_g, g_=g_: e.scalar_tensor_tensor(
                        out=g_[:, :], in0=g_[:, :], scalar=1.0, in1=ps_g[:, 0:CAP], op0=ALU.add, op1=ALU.mult),
                         [ps_g.b, g_.b], [g_.b])
                    S.op("dve", lambda e, ps_u=ps_u, g_=g_, f=f: e.tensor_tensor(out=aTe[:, f, :], in0=ps_u[:, 0:CAP],
                                                                                 in1=g_[:, :], op=ALU.mult),
                         [ps_u.b, g_.b], [aTe.b])

            def down(ex):
                s = ex % 2
                aTe = aT[s]
                for bi in range(NB):
                    y_ = yt[yt_i[0] % 2]
                    st_y = st_ys[yt_i[0] % 2]
                    yt_i[0] += 1
                    for half in range(2):
                        ps_y = psum()
                        for f in range(4):
                            S.op("pe", lambda e, f=f, bi=bi, half=half, ps_y=ps_y: e.matmul(
                                ps_y[:, :], lhsT=aTe[:, f, bi * 128:(bi + 1) * 128],
                                rhs=wd[s][:, f, half * 512:(half + 1) * 512], start=(f == 0), stop=(f == 3)),
                                 [aTe.b, wd[s].b], [ps_y.b], signal=(f == 3))
                        if half == 0:
                            S.op("act", lambda e, ps_y=ps_y, y_=y_: e.activation(out=y_[:, 0:512], in_=ps_y[:, :],
                                                                                 func=AF.Copy), [ps_y.b], [y_.b])
                        else:
                            S.op("dve", lambda e, ps_y=ps_y, y_=y_: e.tensor_copy(out=y_[:, 512:1024], in_=ps_y[:, :]),
                                 [ps_y.b], [y_.b])
                    r0 = ex * CAP + bi * 128
                    S.dma("sp", st_y, lambda e, y_=y_, r0=r0: e.dma_start(out=ysbuf[r0:r0 + 128, :], in_=y_[:, :]),
                          reads=[y_.b])

            do_transposes(0)
            for ex in range(NE):
                gate_up(ex)
                if ex + 1 < NE:
                    do_transposes(ex + 1)
                down(ex)
                if ex + 2 < NE:
                    load_w(ex + 2)
            S.barrier()

        for b_ in x1_tiles:
            b_.w = Tok(st_x1[b_.w.sem is st_x1[1].sem].sem, st_x1[b_.w.sem is st_x1[1].sem].count)
        with contextlib.ExitStack() as p4:
            def sb4(name, shape, dt):
                return T(p4.enter_context(nc.sbuf_tensor("sb_" + name, list(shape), dt)), name)

            NSL = 10
            xl = [sb4("xl%d" % i, [128, D], F32) for i in range(NSL)]
            g1 = [sb4("g1%d" % i, [128, D], BF16) for i in range(NSL)]
            g2 = [sb4("g2%d" % i, [128, D], BF16) for i in range(NSL)]
            st_xl = [S.stream("xl%d" % i) for i in range(NSL)]
            st_g = [S.stream("g%d" % i) for i in range(NSL)]
            st_out = [S.stream("out%d" % i) for i in range(NSL)]

            def fetch(j):
                s = j % NSL
                S.dma("sp", st_xl[s], lambda e, j=j, s=s: e.dma_start(out=xl[s][:, :], in_=x1buf[j * 128:(j + 1) * 128, :]),
                      reads=[x1_tiles[j]], writes=[xl[s].b])
                for kk, gt_ in enumerate((g1[s], g2[s])):
                    S.dma("pool", st_g[s], lambda e, j=j, kk=kk, gt_=gt_: e.indirect_dma_start(
                        out=gt_[:, :], out_offset=None, in_=ysbuf[0:NSLOT, :],
                        in_offset=bass.IndirectOffsetOnAxis(ap=slot_i[:, kk, j:j + 1], axis=0),
                        bounds_check=bc_reg(e), oob_is_err=False), reads=[slot_i.b], writes=[gt_.b])
                S.seal(st_g[s], [g1[s].b, g2[s].b])

            for j in range(min(NSL - 1, NJ)):
                fetch(j)
            for j in range(NJ):
                s = j % NSL
                if j + NSL - 1 < NJ:
                    fetch(j + NSL - 1)
                S.op("dve", lambda e, j=j, s=s: e.scalar_tensor_tensor(out=xl[s][:, :], in0=g1[s][:, :],
                                                                       scalar=wts[:, 0, j:j + 1], in1=xl[s][:, :],
                                                                       op0=ALU.mult, op1=ALU.add),
                     [g1[s].b, wts.b, xl[s].b], [xl[s].b])
                S.op("dve", lambda e, j=j, s=s: e.scalar_tensor_tensor(out=xl[s][:, :], in0=g2[s][:, :],
                                                                       scalar=wts[:, 1, j:j + 1], in1=xl[s][:, :],
                                                                       op0=ALU.mult, op1=ALU.add),
                     [g2[s].b, wts.b, xl[s].b], [xl[s].b])
                S.dma("sp", st_out[s], lambda e, j=j, s=s: e.dma_start(out=y[j * 128:(j + 1) * 128, :], in_=xl[s][:, :]),
                      reads=[xl[s].b])
            S.barrier()
        S.emit()
    return nc


def make_consts(CAP):
    gam = np.array([1.0 - 2.0 ** (-5.0 - h) for h in range(NH)], dtype=np.float64)
    c = np.zeros((128, NCST), dtype=np.float32)
    idx = np.arange(128)
    c[:, C_IDENT:C_IDENT + 128] = np.eye(128, dtype=np.float32)
    kk = idx[:, None]
    qq = idx[None, :]
    dtp = np.zeros((128, 4, 128), dtype=np.float64)
    dq = np.zeros((128, 4, 128), dtype=np.float64)
    for h in range(NH):
        dtp[:, h, :] = (gam[h] ** (-(kk + 1.0))) * (DK ** -0.5) * (qq >= kk)
        dq[:, h, :] = gam[h] ** (qq + 1.0)
        c[:, C_KDEC + h] = (gam[h] ** (127.0 - idx)) * (DK ** -0.5)
    c[:, C_DTP:C_DTP + 512] = dtp.reshape(128, 512)
    c[:, C_DQROW:C_DQROW + 512] = dq.reshape(128, 512)
    c[:, C_U:C_U + 128] = (kk < qq).astype(np.float32)
    c[:, C_EC:C_EC + 32] = (np.arange(32) * CAP)[None, :]
    return c


def rope_tables(pos):
    half = 64
    inv = (np.float32(10000.0) ** (-(np.arange(half, dtype=np.float32)) / np.float32(half))).astype(np.float32)
    ang = pos.astype(np.float32)[:, None] * inv[None, :]
    return np.concatenate([np.cos(ang), np.sin(ang)], axis=1).astype(np.float32)


def prep_core(inputs, b, half, NPREV, NOWN, CAP, seq_own0=None):
    x = inputs["x"]
    n_prev = NPREV * 128
    n_own = NOWN * 128
    own0 = half * n_own if seq_own0 is None else seq_own0
    xc = np.zeros((n_prev + n_own, D), dtype=np.float32)
    cs = np.zeros((n_prev + n_own, 128), dtype=np.float32)
    if own0 > 0:
        xc[:n_prev] = x[b, own0 - n_prev:own0]
        cs[:n_prev] = rope_tables(np.arange(own0 - n_prev, own0))
    xc[n_prev:] = x[b, own0:own0 + n_own]
    cs[n_prev:] = rope_tables(np.arange(own0, own0 + n_own))
    c = make_consts(CAP)
    idx = np.arange(128)
    cb = np.zeros((128, 512), dtype=np.float32)
    cb[:, B_IDENT:B_IDENT + 128] = np.eye(128, dtype=np.float32)
    cb[:, B_MASKC:B_MASKC + 128] = (idx[None, :] >= idx[:, None]).astype(np.float32)
    cb[:, B_MASKP:B_MASKP + 128] = (idx[:, None] > idx[None, :]).astype(np.float32)
    if own0 > 0:
        cb[:, B_MASKP0:B_MASKP0 + 128] = (idx[:, None] > idx[None, :]).astype(np.float32)
    c[:, C_GMIX:C_GMIX + 8] = inputs["norm_mix_g"][0].reshape(8, 128).T
    c[:, C_GN:C_GN + 8] = inputs["ret_gn_g"][0].reshape(8, 128).T
    c[:, C_GFFN:C_GFFN + 8] = inputs["norm_ffn_g"][0].reshape(8, 128).T
    c[:, C_GQ:C_GQ + 64] = inputs["q_norm_g"][0][None, :]
    c[:, C_GK:C_GK + 64] = inputs["k_norm_g"][0][None, :]
    c[:, C_SINK:C_SINK + 8] = inputs["sinks"][0][None, :]
    c[:, C_BR:C_BR + 4] = inputs["b_router_group"][0][None, :]
    c[:, C_BR + 4:C_BR + 36] = inputs["b_router_expert"][0][None, :]
    return {"xc": xc, "cs": cs, "cst": c, "cstb": cb,
            "gffn": np.ascontiguousarray(inputs["norm_ffn_g"][0][None, :])}


def shared_inputs(inputs):
    wr = np.concatenate([inputs["w_router_group"][0], inputs["w_router_expert"][0]], axis=1)
    return {
        "w_in": np.ascontiguousarray(inputs["w_in"][0]),
        "w_ret_o": np.ascontiguousarray(inputs["w_ret_o"][0]),
        "w_swa_o": np.ascontiguousarray(inputs["w_swa_o"][0]),
        "w_out": np.ascontiguousarray(inputs["w_out"][0]),
        "wr": np.ascontiguousarray(wr),
        "w_gate": np.ascontiguousarray(inputs["w_gate"][0]),
        "w_up": np.ascontiguousarray(inputs["w_up"][0]),
        "w_down": np.ascontiguousarray(inputs["w_down"][0]),
    }


CAP_DEFAULT = 384
ORDER = "fbbfbbfbbbffbffbbbfbbfbbbfbbffbbbbfbf"
LAST_COUNTS = {}


def kernel(**inputs):
    inputs = {k: np.asarray(v) for k, v in inputs.items()}
    B, SEQ, _ = inputs["x"].shape
    NOWN = SEQ // 2 // 128
    NPREV = NOWN
    nc = build(NPREV=NPREV, NOWN=NOWN, CAP=CAP_DEFAULT, moe=True)
    sh = shared_inputs(inputs)
    in_maps = []
    for core in range(8):
        b, half = core // 2, core % 2
        m = prep_core(inputs, b, half, NPREV, NOWN, CAP_DEFAULT)
        m.update(sh)
        in_maps.append(m)
    res = run_bass_kernel_spmd(nc, in_maps, core_ids=list(range(8)))
    out = np.zeros((B, SEQ, D), dtype=np.float32)
    n_own = NOWN * 128
    for core in range(8):
        b, half = core // 2, core % 2
        out[b, half * n_own:(half + 1) * n_own] = res.results[core]["y"]
    return out
```

```python
import contextlib
import numpy as np
import concourse.bass as bass
import concourse.mybir as mybir
from concourse.bass_utils import run_bass_kernel_spmd

F32 = mybir.dt.float32
BF16 = mybir.dt.bfloat16
I32 = mybir.dt.int32
AF = mybir.ActivationFunctionType
ALU = mybir.AluOpType
AX = mybir.AxisListType

D = 1024
NH = 4
DK = 128
DV = 256
INW = 5888
NE = 32
DFF = 512
EPS = 1e-6
O_RQ, O_RK, O_RV, O_RG, O_SQ, O_SK, O_SV, O_GA, O_GB = 0, 512, 1024, 2048, 3072, 3584, 3712, 3840, 4864

C_IDENT = 0
C_DTP = 128
C_DQROW = 640
C_U = 1152
C_KDEC = 1280
C_EC = 1284
C_GMIX = 1316
C_GN = 1324
C_GFFN = 1332
C_GQ = 1340
C_GK = 1404
C_SINK = 1468
C_BR = 1476
C_GFFNBC = 1512
B_IDENT, B_MASKC, B_MASKP, B_MASKP0 = 0, 128, 256, 384
NCST = C_GFFNBC


class Tok:
    __slots__ = ("sem", "val")

    def __init__(self, sem, val):
        self.sem = sem
        self.val = val


class Buf:
    __slots__ = ("name", "w", "r", "lock")

    def __init__(self, name):
        self.name = name
        self.w = None
        self.r = {}
        self.lock = None


class Eng:
    def __init__(self, name, sem):
        self.name = name
        self.sem = sem
        self.n = 0
        self.known = {}
        self.prog = []


class Stream:
    def __init__(self, sem):
        self.sem = sem
        self.count = 0


class Sched:
    def __init__(self, nc, stack):
        self.nc = nc
        self.stack = stack
        self.eng = {}
        for nm in ("pe", "act", "dve", "pool", "sp"):
            self.eng[nm] = Eng(nm, stack.enter_context(nc.semaphore("s_" + nm)))
        self.streams = []

    def stream(self, name):
        s = Stream(self.stack.enter_context(self.nc.semaphore("d_" + name)))
        self.streams.append(s)
        return s

    def _deps(self, E, reads, writes):
        deps = []
        for b in reads:
            if b.w is not None:
                deps.append((b.w, True))
        for b in writes:
            if b.w is not None:
                deps.append((b.w, False))
            for t in b.r.values():
                deps.append((t, False))
        for tok, raw in deps:
            if tok.sem is E.sem:
                if E.name == "pe":
                    continue
                assert tok.val <= E.n, "same-engine dependency on unsignaled op"
            if E.known.get(id(tok.sem), 0) >= tok.val:
                continue
            E.prog.append(("wait", tok.sem, tok.val))
            E.known[id(tok.sem)] = tok.val

    def _mark(self, tok, reads, writes):
        for b in writes:
            b.w = tok
            b.r = {}
        for b in reads:
            old = b.r.get(id(tok.sem))
            if old is None or old.val < tok.val:
                b.r[id(tok.sem)] = tok

    def op(self, e, fn, reads=(), writes=(), signal=True):
        E = self.eng[e]
        if e != "pe":
            locks = [b.lock for b in reads if b.lock is not None]
            if locks:
                writes = list(writes) + locks
        self._deps(E, reads, writes)
        if signal:
            E.n += 1
            tok = Tok(E.sem, E.n)
        else:
            tok = Tok(E.sem, E.n + 1)
        E.prog.append(("op", fn, signal))
        self._mark(tok, reads, writes)
        return tok

    def dma(self, q, st, fn, reads=(), writes=()):
        E = self.eng[q]
        self._deps(E, reads, writes)
        st.count += 16
        tok = Tok(st.sem, st.count)
        E.prog.append(("dma", fn, st.sem))
        self._mark(tok, reads, writes)
        return tok

    def seal(self, st, bufs):
        for b in bufs:
            b.w = Tok(st.sem, st.count)

    def barrier(self):
        toks = [Tok(E.sem, E.n) for E in self.eng.values() if E.n > 0]
        toks += [Tok(s.sem, s.count) for s in self.streams if s.count > 0]
        for E in self.eng.values():
            for t in toks:
                if t.sem is E.sem:
                    continue
                if E.known.get(id(t.sem), 0) >= t.val:
                    continue
                E.prog.append(("wait", t.sem, t.val))
                E.known[id(t.sem)] = t.val

    def emit(self):
        nc = self.nc
        sch = self

        def run(engobj, E):
            for item in E.prog:
                if item[0] == "wait":
                    engobj.wait_ge(item[1], item[2])
                elif item[0] == "op":
                    ins = item[1](engobj)
                    if item[2]:
                        ins.then_inc(E.sem, 1)
                else:
                    ins = item[1](engobj)
                    ins.then_inc(item[2], 16)

        with nc.Block() as blk:
            @blk.tensor
            def _(e):
                run(e, sch.eng["pe"])

            @blk.scalar
            def _(e):
                run(e, sch.eng["act"])

            @blk.vector
            def _(e):
                run(e, sch.eng["dve"])

            @blk.gpsimd
            def _(e):
                run(e, sch.eng["pool"])

            @blk.sync
            def _(e):
                run(e, sch.eng["sp"])


class T:
    def __init__(self, t, name):
        self.t = t
        self.b = Buf(name)

    def __getitem__(self, k):
        return self.t[k]


def build(NPREV=32, NOWN=32, CAP=384, moe=True, dbg_stop=99):
    nc = bass.Bass("TRN2", target_bir_lowering=False)
    NT = NPREV + NOWN
    NTOK = NOWN * 128
    NSLOT = NE * CAP
    XS_ROWS = NSLOT + 128

    def din(name, shape, dt=F32):
        return nc.dram_tensor(name, list(shape), dt, kind="ExternalInput").ap()

    xc = din("xc", [NT * 128, D])
    cs = din("cs", [NT * 128, 128])
    cst = din("cst", [128, NCST])
    cstb = din("cstb", [128, 512])
    gffn = din("gffn", [1, D])
    w_in = din("w_in", [D, INW])
    w_ret_o = din("w_ret_o", [D, D])
    w_swa_o = din("w_swa_o", [512, D])
    w_out = din("w_out", [D, D])
    wr = din("wr", [D, 36])
    w_gate = din("w_gate", [NE, D, DFF])
    w_up = din("w_up", [NE, D, DFF])
    w_down = din("w_down", [NE, DFF, D])
    y = nc.dram_tensor("y", [NTOK, D], F32, kind="ExternalOutput").ap()
    x1buf = nc.dram_tensor("x1buf", [NTOK, D], F32, kind="Internal").ap()
    h2buf = nc.dram_tensor("h2buf", [NTOK, D], BF16, kind="Internal").ap()
    xsbuf = nc.dram_tensor("xsbuf", [XS_ROWS, D], BF16, kind="Internal").ap()
    ysbuf = nc.dram_tensor("ysbuf", [XS_ROWS, D], BF16, kind="Internal").ap()

    gam = [1.0 - 2.0 ** (-5.0 - h) for h in range(NH)]
    cdec = [g ** 128 for g in gam]

    with contextlib.ExitStack() as stack:
        S = Sched(nc, stack)

        def sb(name, shape, dt):
            return T(stack.enter_context(nc.sbuf_tensor("sb_" + name, list(shape), dt)), name)

        banks = [T(stack.enter_context(nc.psum_tensor("ps%d" % i, [128, 512], F32)), "ps%d" % i)
                 for i in range(8)]
        for bk in banks:
            bk.b.lock = Buf(bk.b.name + "_lock")
        bank_i = [0]

        ring_i = {"f": 0, "b": 0}

        def psum(th=None):
            if th is None:
                b = banks[bank_i[0] % 8]
                bank_i[0] += 1
                return b
            assert free_banks, "PSUM pool exhausted"
            b = free_banks.pop(0)
            held[th].append(b)
            return b

        free_banks = list(banks)
        held = {"f": [], "b": []}

        def release(th):
            free_banks.extend(held[th])
            held[th] = []

        C = sb("cst", [128, NCST], F32)
        CB = sb("cstb", [128, 512], BF16)

        class _View:
            def __init__(self, t, off, b):
                self.t, self.off, self.b = t, off, b

            def __getitem__(self, k):
                return self.t[:, self.off:self.off + 128][k]

        identb = _View(CB, B_IDENT, CB.b)
        maskC = _View(CB, B_MASKC, CB.b)
        maskP = _View(CB, B_MASKP, CB.b)
        maskP0 = _View(CB, B_MASKP0, CB.b)
        ones_b = sb("ones_b", [128, 128], BF16)
        gqk = sb("gqk", [128, 64], F32)
        negc = sb("negc", [128, 1], F32)
        sinkexp = sb("sinkexp", [128, 8], F32)
        wrs = sb("wrs", [128, 8, 36], F32)
        L = sb("L", [128, NOWN, 36], F32)
        small = sb("small", [128, 64], F32)
        nhalf = sb("nhalf", [128, 8], F32)
        S.op("pool", lambda e: e.memset(nhalf[:, :], -0.5), [], [nhalf.b])

        st_c = S.stream("cst")
        S.dma("sp", st_c, lambda e: e.dma_start(out=C[:, :], in_=cst), writes=[C.b])
        S.dma("sp", st_c, lambda e: e.dma_start(out=wrs[:, :, :], in_=wr.rearrange("(k p) n -> p k n", p=128)),
              writes=[wrs.b])
        S.seal(st_c, [C.b, wrs.b])
        st_cb = S.stream("cstb")
        S.dma("pool", st_cb, lambda e: e.dma_start(out=CB[:, :], in_=cstb), writes=[CB.b])
        S.op("dve", lambda e: e.memset(ones_b[:, :], 1.0), [], [ones_b.b])
        S.op("dve", lambda e: e.scalar_tensor_tensor(out=gqk[:, :], in0=C[:, C_GQ:C_GQ + 64], scalar=0.125,
                                                     in1=C[:, C_GK:C_GK + 64], op0=ALU.mult, op1=ALU.mult),
             [C.b], [gqk.b])
        S.op("dve", lambda e: e.tensor_reduce(out=small[:, 0:1], in_=gqk[:, :], axis=AX.X, op=ALU.max,
                                              apply_absolute_value=True), [gqk.b], [small.b])
        S.op("dve", lambda e: e.tensor_scalar(out=negc[:, :], in0=small[:, 0:1], scalar1=-64.0, scalar2=None,
                                              op0=ALU.mult), [small.b], [negc.b])
        S.op("act", lambda e: e.activation(out=sinkexp[:, :], in_=C[:, C_SINK:C_SINK + 8], func=AF.Exp,
                                           bias=negc[:, 0:1], scale=1.0), [C.b, negc.b], [sinkexp.b])
        for k in range(8):
            S.op("dve", lambda e, k=k: e.tensor_scalar(out=wrs[:, k, :], in0=wrs[:, k, :],
                                                       scalar1=C[:, C_GFFN + k:C_GFFN + k + 1], scalar2=None,
                                                       op0=ALU.mult), [C.b, wrs.b], [wrs.b])

        with contextlib.ExitStack() as p1:
            def sb1(name, shape, dt):
                return T(p1.enter_context(nc.sbuf_tensor("sb_" + name, list(shape), dt)), name)

            class TV:
                def __init__(self, ap, b):
                    self.ap, self.b = ap, b

                def __getitem__(self, k):
                    return self.ap[k]

            Win = sb1("Win", [128, 8, INW], BF16)
            Wro = sb1("Wro", [128, 8, D], BF16)
            Wso = sb1("Wso", [128, 4, D], BF16)
            Wout = sb1("Wout", [128, 8, D], BF16)
            WA0, WA1 = O_RK, O_RG
            WinA = Buf("WinA")
            WinB = Buf("WinB")
            st_wa = S.stream("w1a")
            st_w = S.stream("w1")
            for k in range(8):
                S.dma("pool", st_wa, lambda e, k=k: e.dma_start(out=Win[:, k, WA0:WA1],
                                                                  in_=w_in[k * 128:(k + 1) * 128, WA0:WA1]),
                      writes=[WinA])
            S.seal(st_wa, [WinA])
            wq = []
            for k in range(8):
                wq.append(lambda k=k: S.dma("pool", st_w, lambda e: e.dma_start(
                    out=Win[:, k, 0:WA0], in_=w_in[k * 128:(k + 1) * 128, 0:WA0]), writes=[WinB]))
                wq.append(lambda k=k: S.dma("pool", st_w, lambda e: e.dma_start(
                    out=Win[:, k, WA1:INW], in_=w_in[k * 128:(k + 1) * 128, WA1:INW], max_dma_last_dim=7680),
                    writes=[WinB]))
            wq.append(lambda: S.dma("pool", st_w, lambda e: e.dma_start(
                out=Wro[:, :, :], in_=w_ret_o.rearrange("(k p) n -> p k n", p=128)), writes=[Wro.b]))
            wq.append(lambda: S.dma("pool", st_w, lambda e: e.dma_start(
                out=Wso[:, :, :], in_=w_swa_o.rearrange("(k p) n -> p k n", p=128)), writes=[Wso.b]))
            wq.append(lambda: S.dma("pool", st_w, lambda e: e.dma_start(
                out=Wout[:, :, :], in_=w_out.rearrange("(k p) n -> p k n", p=128)), writes=[Wout.b]))

            def issue_weights(n):
                for _ in range(n):
                    if wq:
                        wq.pop(0)()
                if not wq:
                    S.seal(st_w, [WinB, Wro.b, Wso.b, Wout.b])
            S.op("dve", lambda e: e.tensor_scalar(out=small[:, 8:16], in0=C[:, C_GN:C_GN + 8], scalar1=0.5,
                                                  scalar2=None, op0=ALU.mult), [C.b], [small.b])

            def fold_win(c0, c1, wb):
                for k in range(8):
                    if k % 2 == 0:
                        S.op("dve", lambda e, k=k: e.tensor_scalar(out=Win[:, k, c0:c1], in0=Win[:, k, c0:c1],
                                                                   scalar1=C[:, C_GMIX + k:C_GMIX + k + 1], scalar2=None,
                                                                   op0=ALU.mult), [C.b, wb], [wb])
                    else:
                        S.op("act", lambda e, k=k: e.activation(out=Win[:, k, c0:c1], in_=Win[:, k, c0:c1], func=AF.Copy,
                                                                scale=C[:, C_GMIX + k:C_GMIX + k + 1]), [C.b, wb], [wb])

            fold_win(WA0, WA1, WinA)

            def fold_rest():
                fold_win(0, WA0, WinB)
                fold_win(WA1, INW, WinB)
                for k in range(8):
                    if k % 2 == 1:
                        S.op("dve", lambda e, k=k: e.tensor_scalar(out=Wro[:, k, :], in0=Wro[:, k, :],
                                                                   scalar1=small[:, 8 + k:9 + k], scalar2=None,
                                                                   op0=ALU.mult), [small.b, Wro.b], [Wro.b])
                    else:
                        S.op("act", lambda e, k=k: e.activation(out=Wro[:, k, :], in_=Wro[:, k, :], func=AF.Copy,
                                                                scale=small[:, 8 + k:9 + k]), [small.b, Wro.b], [Wro.b])
                S.op("dve", lambda e: e.tensor_scalar(out=Wout[:, 0:4, :], in0=Wout[:, 0:4, :], scalar1=0.5, scalar2=None,
                                                      op0=ALU.mult), [Wout.b], [Wout.b])
                S.op("act", lambda e: e.activation(out=Wout[:, 4:8, :], in_=Wout[:, 4:8, :], func=AF.Copy, scale=0.5),
                     [Wout.b], [Wout.b])

            xt = [sb1("xt%d" % i, [128, D], F32) for i in range(2)]
            cst_t = [sb1("cs%d" % i, [128, 128], F32) for i in range(2)]
            st_x = [S.stream("x%d" % i) for i in range(2)]
            h = sb1("h", [128, D], BF16)
            qrot = sb1("qrot", [128, 4, 2, 64], BF16)
            krot = sb1("krot", [128, 4, 2, 64], BF16)
            rt = [sb1("rt%d" % i, [128, 4, 64], BF16) for i in range(4)]
            v = sb1("v", [128, D], BF16)
            sgt = [sb1("sgt%d" % i, [128, 512], BF16) for i in range(2)]
            qT = sb1("qT", [128, 4, 128], BF16)
            kT = sb1("kT", [128, 4, 128], BF16)
            kdec = sb1("kdec", [128, 4, 128], BF16)
            SdT = sb1("SdT", [128, 4, 128], BF16)
            R = sb1("R", [128, 4, DV], F32)
            Rb = sb1("Rb", [128, 4, DV], BF16)
            gnt = sb1("gnt", [128, D], BF16)
            qn = sb1("qn", [128, 4, 2, 64], BF16)
            kn = sb1("kn", [128, 128], BF16)
            qnT = sb1("qnT", [128, 4, 128], BF16)
            sqk = kn
            sqq = TV(qn.t[:, :, :, :].rearrange("p i j d -> p (i j d)"), qn.b)
            kTr = [sb1("kTr%d" % i, [128, 128], BF16) for i in range(3)]
            vsr = [sb1("vsr%d" % i, [128, 2, 65], BF16) for i in range(3)]
            eb = [sb1("eb%d" % i, [128, 512], BF16) for i in range(4)]
            on = sb1("on", [128, 8, 64], BF16)
            stat = sb1("stat", [128, 64], F32)
            statp = sb1("statp", [128, 32], F32)
            hT2 = [sb1("hT%d" % i, [128, 8, 128], BF16) for i in range(2)]
            ret2 = [sb1("ret%d" % i, [128, D], BF16) for i in range(2)]
            oTn1 = sb1("oTn", [128, 4, 128], BF16)
            oTn2 = [oTn1, oTn1]
            xT2 = sb1("xT2", [128, 8, 128], BF16)
            ma = sb1("ma", [128, D], BF16)
            tb = sb1("tb", [128, D], BF16)
            sgb = [sb1("sgb%d" % i, [128, 512], BF16) for i in range(4)]
            x1T = sb1("x1T", [128, 4, 128], F32)
            statb = sb1("statb", [128, 48], F32)
            rr2 = [sb1("rr%d" % i, [128, 2], F32) for i in range(2)]
            h2 = ma
            eb_i = [0]
            sg_i = [0]
            sgb_i = [0]

            S.op("dve", lambda e: e.memset(R[:, :, :], 0.0), [], [R.b])
            S.op("pool", lambda e: e.memset(Rb[:, :, :], 0.0), [], [Rb.b])
            for i_ in range(3):
                S.op("pool", lambda e, i_=i_: e.memset(vsr[i_][:, :, :], 1.0), [], [vsr[i_].b])

            st_z = S.stream("zfill")

            def zero_fill():
                if not moe:
                    return
                S.op("pool", lambda e: e.memset(gnt[:, :], 0.0), [], [gnt.b])
                nblk = XS_ROWS // 128
                for b0 in range(0, nblk, 16):
                    nb = min(16, nblk - b0)
                    S.dma("sp", st_z, lambda e, b0=b0, nb=nb: e.dma_start(
                        out=xsbuf[b0 * 128:(b0 + nb) * 128, :].rearrange("(n p) d -> p n d", p=128),
                        in_=gnt[:, :].unsqueeze(1).broadcast_to([128, nb, D])), reads=[gnt.b])

            st_x1 = [S.stream("x1st%d" % i) for i in range(2)]
            st_h2 = S.stream("h2st")
            x1_tiles = [Buf("x1d%d" % j) for j in range(NOWN)]
            h2_tiles = [Buf("h2d%d" % j) for j in range(NOWN)]

            def load_tile(ci):
                s = ci % 2
                S.dma("sp", st_x[s], lambda e: e.dma_start(out=xt[s][:, :], in_=xc[ci * 128:(ci + 1) * 128, :]),
                      writes=[xt[s].b])
                S.dma("sp", st_x[s], lambda e: e.dma_start(out=cst_t[s][:, :], in_=cs[ci * 128:(ci + 1) * 128, :]),
                      writes=[cst_t[s].b])
                S.seal(st_x[s], [xt[s].b, cst_t[s].b])

            def inproj(th, hT, col0, ncol):
                ps = psum(th)
                for k in range(8):
                    S.op("pe", lambda e, k=k, ps=ps: e.matmul(ps[:, 0:ncol], lhsT=hT[:, k, :],
                                                              rhs=Win[:, k, col0:col0 + ncol],
                                                              start=(k == 0), stop=(k == 7)),
                         [hT.b, WinA if WA0 <= col0 < WA1 else WinB], [ps.b], signal=(k == 7))
                return ps

            def transposes(src, nblk, ps, rbufs, dst_off=0, dt=BF16, ident=None):
                ident = ident or identb
                pv = ps[:, :].bitcast(dt) if dt == BF16 else ps[:, :]
                for k in range(nblk):
                    S.op("pe", lambda e, k=k: e.transpose(pv[:, dst_off + k * 128:dst_off + (k + 1) * 128],
                                                          src(k), ident[:, :]),
                         list(rbufs) + [ident.b], [ps.b], signal=(k == nblk - 1))

            def rotary(ps, cst_s, dst, rt, r_ap, rb):
                pv = ps[:, :].rearrange("p (h t d) -> p h t d", h=4, t=2, d=64)
                cosb = cst_s[:, 0:64].unsqueeze(1).broadcast_to([128, 4, 64])
                sinb = cst_s[:, 64:128].unsqueeze(1).broadcast_to([128, 4, 64])
                S.op("dve", lambda e: e.scalar_tensor_tensor(out=rt[0][:, :, :], in0=pv[:, :, 0, :], scalar=r_ap,
                                                             in1=cosb, op0=ALU.mult, op1=ALU.mult),
                     [ps.b, cst_s.b, rb], [rt[0].b])
                S.op("dve", lambda e: e.scalar_tensor_tensor(out=rt[1][:, :, :], in0=pv[:, :, 1, :], scalar=r_ap,
                                                             in1=sinb, op0=ALU.mult, op1=ALU.mult),
                     [ps.b, cst_s.b, rb], [rt[1].b])
                S.op("dve", lambda e: e.tensor_tensor(out=dst[:, :, 0, :], in0=rt[0][:, :, :], in1=rt[1][:, :, :],
                                                       op=ALU.subtract), [rt[0].b, rt[1].b], [dst.b])
                S.op("dve", lambda e: e.scalar_tensor_tensor(out=rt[2][:, :, :], in0=pv[:, :, 0, :], scalar=r_ap,
                                                             in1=sinb, op0=ALU.mult, op1=ALU.mult),
                     [ps.b, cst_s.b, rb], [rt[2].b])
                S.op("dve", lambda e: e.scalar_tensor_tensor(out=rt[3][:, :, :], in0=pv[:, :, 1, :], scalar=r_ap,
                                                             in1=cosb, op0=ALU.mult, op1=ALU.mult),
                     [ps.b, cst_s.b, rb], [rt[3].b])
                S.op("dve", lambda e: e.tensor_tensor(out=dst[:, :, 1, :], in0=rt[2][:, :, :], in1=rt[3][:, :, :],
                                                       op=ALU.add), [rt[2].b, rt[3].b], [dst.b])

            def rms_rstd(ss_ap, out_ap, n, inv_n, reads, wbuf):
                S.op("dve", lambda e: e.tensor_scalar(out=out_ap, in0=ss_ap, scalar1=inv_n, scalar2=EPS,
                                                       op0=ALU.mult, op1=ALU.add), reads, [wbuf])
                S.op("pool", lambda e: e.tensor_tensor(out=out_ap, in0=out_ap, in1=nhalf[:, 0:n], op=ALU.pow),
                     [wbuf, nhalf.b], [wbuf])

            def front(ci, bs=None):
                full = ci >= NPREV
                last_prev = (ci == NPREV - 1)
                s = ci % 2
                x_s, c_s = xt[s], cst_t[s]
                hT = hT2[s]
                h_, krot_, rt_, v_, kdec_, stat_, th = bs if bs is not None else (h, krot, rt, v, kdec, stat, "f")

                def gate_pre(half):
                    ps_g = inproj(th, hT, O_RG + half * 512, 512)
                    sg = sgt[half]
                    S.op("act", lambda e, ps_g=ps_g, sg=sg: e.activation(out=sg[:, :], in_=ps_g[:, :], func=AF.Tanh,
                                                                         scale=rr2[s][:, 1:2]), [ps_g.b, rr2[s].b], [sg.b])
                    S.op("dve", lambda e, ps_g=ps_g, sg=sg: e.scalar_tensor_tensor(
                        out=sg[:, :], in0=sg[:, :], scalar=1.0, in1=ps_g[:, :], op0=ALU.add, op1=ALU.mult),
                         [ps_g.b, sg.b], [sg.b])
                rr = rr2[s]
                r_ap = rr[:, 0:1]
                rh_ap = rr[:, 1:2]
                S.op("act", lambda e: e.activation(out=h_[:, :], in_=x_s[:, :], func=AF.Square,
                                                   accum_out=stat_[:, 0:1]), [x_s.b], [h_.b, stat_.b])
                rms_rstd(stat_[:, 0:1], rr[:, 0:1], 1, 1.0 / D, [stat_.b], rr.b)
                S.op("dve", lambda e: e.tensor_scalar(out=rr[:, 1:2], in0=rr[:, 0:1], scalar1=0.5, scalar2=None,
                                                      op0=ALU.mult), [rr.b], [rr.b])
                ps_xa, ps_xb = psum(th), psum(th)
                for k in range(8):
                    pp = ps_xa if k < 4 else ps_xb
                    S.op("pe", lambda e, k=k, pp=pp: e.transpose(pp[:, (k % 4) * 128:(k % 4 + 1) * 128],
                                                                 x_s[:, k * 128:(k + 1) * 128],
                                                                 C[:, C_IDENT:C_IDENT + 128]),
                         [x_s.b, C.b], [pp.b], signal=(k % 4 == 3))
                S.op("act", lambda e: e.activation(out=hT[:, 0:4, :].rearrange("p k t -> p (k t)"), in_=ps_xa[:, :],
                                                   func=AF.Copy), [ps_xa.b], [hT.b])
                S.op("dve", lambda e: e.tensor_copy(out=hT[:, 4:8, :].rearrange("p k t -> p (k t)"), in_=ps_xb[:, :]),
                     [ps_xb.b], [hT.b])
                yield th
                ps_k = inproj(th, hT, O_RK, 512)
                rotary(ps_k, c_s, krot_, rt_, r_ap, rr.b)
                yield th
                ps_v1 = inproj(th, hT, O_RV, 512)
                S.op("act", lambda e: e.activation(out=v_[:, 0:512], in_=ps_v1[:, :], func=AF.Copy, scale=r_ap),
                     [ps_v1.b, rr.b], [v_.b])
                yield th
                ps_v2 = inproj(th, hT, O_RV + 512, 512)
                S.op("act", lambda e: e.activation(out=v_[:, 512:1024], in_=ps_v2[:, :], func=AF.Copy, scale=r_ap),
                     [ps_v2.b, rr.b], [v_.b])
                yield th
                if full:
                    ps_q = inproj(th, hT, O_RQ, 512)
                    rotary(ps_q, c_s, qrot, rt_, r_ap, rr.b)
                S.op("dve", lambda e: e.tensor_tensor(
                    out=kdec_[:, :, :], in0=krot_[:, :, :, :].rearrange("p h t d -> p h (t d)"),
                    in1=C[:, C_KDEC:C_KDEC + 4].unsqueeze(2).broadcast_to([128, 4, 128]), op=ALU.mult),
                     [krot_.b, C.b], [kdec_.b])
                yield th
                if full:
                    gate_pre(0)
                    yield th
                    ps_t = psum(th)
                    ptv = ps_t[:, :].bitcast(BF16)
                    for hh in range(4):
                        S.op("pe", lambda e, hh=hh: e.transpose(ptv[:, hh * 128:(hh + 1) * 128],
                                                                qrot[:, hh, :, :].rearrange("p t d -> p (t d)"),
                                                                identb[:, :]), [qrot.b, CB.b], [ps_t.b], signal=False)
                    for hh in range(4):
                        S.op("pe", lambda e, hh=hh: e.transpose(ptv[:, 512 + hh * 128:512 + (hh + 1) * 128],
                                                                krot_[:, hh, :, :].rearrange("p t d -> p (t d)"),
                                                                identb[:, :]), [krot_.b, CB.b], [ps_t.b], signal=(hh == 3))
                    S.op("dve", lambda e: e.tensor_tensor(out=qT[:, :, :].rearrange("p h t -> p (h t)"),
                                                          in0=ptv[:, 0:512], in1=C[:, C_DQROW:C_DQROW + 512],
                                                          op=ALU.mult), [ps_t.b, C.b], [qT.b])
                    S.op("dve", lambda e: e.tensor_copy(out=kT[:, :, :].rearrange("p h t -> p (h t)"),
                                                        in_=ptv[:, 512:1024]), [ps_t.b], [kT.b])
                    yield th
                    ps_s = psum(th)
                    for hh in range(4):
                        S.op("pe", lambda e, hh=hh: e.matmul(ps_s[:, hh * 128:(hh + 1) * 128], lhsT=kT[:, hh, :],
                                                             rhs=qT[:, hh, :], start=True, stop=True),
                             [kT.b, qT.b], [ps_s.b], signal=(hh == 3))
                    S.op("dve", lambda e: e.tensor_tensor(out=SdT[:, :, :].rearrange("p h t -> p (h t)"),
                                                          in0=ps_s[:, :], in1=C[:, C_DTP:C_DTP + 512], op=ALU.mult),
                         [ps_s.b, C.b], [SdT.b])
                    yield th
                    ps_o = [psum(th), psum(th)]
                    for hh in range(4):
                        po = ps_o[hh // 2]
                        c0 = (hh % 2) * 256
                        S.op("pe", lambda e, hh=hh, po=po, c0=c0: e.matmul(
                            po[:, c0:c0 + 256], lhsT=SdT[:, hh, :], rhs=v_[:, hh * 256:(hh + 1) * 256],
                            start=True, stop=False), [SdT.b, v_.b], [po.b], signal=False)
                        S.op("pe", lambda e, hh=hh, po=po, c0=c0: e.matmul(
                            po[:, c0:c0 + 256], lhsT=qT[:, hh, :], rhs=Rb[:, hh, :],
                            start=False, stop=True), [qT.b, Rb.b], [po.b], signal=(hh % 2 == 1))
                    for hh in range(4):
                        po = ps_o[hh // 2]
                        c0 = (hh % 2) * 256
                        S.op("dve", lambda e, hh=hh, po=po, c0=c0: e.bn_stats(out=stat_[:, 32 + hh * 6:38 + hh * 6],
                                                                              in_=po[:, c0:c0 + 256]),
                             [po.b], [stat_.b])
                        S.op("dve", lambda e, hh=hh: e.bn_aggr(out=stat_[:, 56 + hh * 2:58 + hh * 2],
                                                               in_=stat_[:, 32 + hh * 6:38 + hh * 6]), [stat_.b], [stat_.b])
                    var_ap = stat_[:, 56:64].rearrange("p (h t) -> p h t", t=2)[:, :, 1]
                    mean_ap = stat_[:, 56:64].rearrange("p (h t) -> p h t", t=2)[:, :, 0]
                    S.op("dve", lambda e: e.tensor_scalar(out=stat_[:, 2:6], in0=var_ap, scalar1=EPS, scalar2=None,
                                                           op0=ALU.add), [stat_.b], [stat_.b])
                    S.op("pool", lambda e: e.tensor_tensor(out=stat_[:, 2:6], in0=stat_[:, 2:6], in1=nhalf[:, 0:4],
                                                           op=ALU.pow), [stat_.b, nhalf.b], [stat_.b])
                    S.op("dve", lambda e: e.tensor_scalar(out=stat_[:, 2:6], in0=stat_[:, 2:6], scalar1=r_ap, scalar2=None,
                                                          op0=ALU.mult), [stat_.b, rr.b], [stat_.b])
                    S.op("dve", lambda e: e.scalar_tensor_tensor(out=stat_[:, 28:32], in0=mean_ap, scalar=-1.0,
                                                                 in1=stat_[:, 2:6], op0=ALU.mult, op1=ALU.mult),
                         [stat_.b], [stat_.b])
                    for hh in range(4):
                        po = ps_o[hh // 2]
                        c0 = (hh % 2) * 256
                        S.op("act", lambda e, hh=hh, po=po, c0=c0: e.activation(
                            out=gnt[:, hh * 256:(hh + 1) * 256], in_=po[:, c0:c0 + 256], func=AF.Identity,
                            bias=stat_[:, 28 + hh:29 + hh], scale=stat_[:, 2 + hh:3 + hh]), [po.b, stat_.b], [gnt.b])
                yield th
                ps_kv = [psum(th), psum(th)]
                for hh in range(4):
                    pk = ps_kv[hh // 2]
                    c0 = (hh % 2) * 256
                    S.op("pe", lambda e, hh=hh, pk=pk, c0=c0: e.matmul(
                        pk[:, c0:c0 + 256], lhsT=kdec_[:, hh, :], rhs=v_[:, hh * 256:(hh + 1) * 256],
                        start=True, stop=True), [kdec_.b, v_.b], [pk.b], signal=(hh % 2 == 1))
                for hh in range(4):
                    pk = ps_kv[hh // 2]
                    c0 = (hh % 2) * 256
                    S.op("dve", lambda e, hh=hh, pk=pk, c0=c0: e.scalar_tensor_tensor(
                        out=R[:, hh, :], in0=R[:, hh, :], scalar=cdec[hh], in1=pk[:, c0:c0 + 256],
                        op0=ALU.mult, op1=ALU.add), [pk.b, R.b], [R.b])
                S.op("act", lambda e: e.activation(out=Rb[:, :, :], in_=R[:, :, :], func=AF.Copy), [R.b], [Rb.b])
                yield th
                if full or last_prev:
                    rs = ci % 3
                    ps_kv2 = inproj(th, hT, O_SK, 256)
                    S.op("act", lambda e: e.activation(out=sqk[:, :], in_=ps_kv2[:, 0:128], func=AF.Square, scale=r_ap),
                         [ps_kv2.b, rr.b], [sqk.b])
                    S.op("act", lambda e: e.activation(out=vsr[rs][:, :, 0:64],
                                                       in_=ps_kv2[:, 128:256].rearrange("p (j d) -> p j d", j=2),
                                                       func=AF.Copy, scale=r_ap), [ps_kv2.b, rr.b], [vsr[rs].b])
                    S.op("dve", lambda e: e.tensor_reduce(out=stat_[:, 16:18],
                                                          in_=sqk[:, :].rearrange("p (h d) -> p h d", h=2),
                                                          axis=AX.X, op=ALU.add), [sqk.b], [stat_.b])
                    rms_rstd(stat_[:, 16:18], stat_[:, 18:20], 2, 1.0 / 64, [stat_.b], stat_.b)
                    S.op("dve", lambda e: e.tensor_scalar(out=stat_[:, 18:20], in0=stat_[:, 18:20], scalar1=r_ap,
                                                          scalar2=None, op0=ALU.mult), [stat_.b, rr.b], [stat_.b])
                    for hk in range(2):
                        S.op("dve", lambda e, hk=hk: e.scalar_tensor_tensor(
                            out=kn[:, hk * 64:(hk + 1) * 64], in0=ps_kv2[:, hk * 64:(hk + 1) * 64],
                            scalar=stat_[:, 18 + hk:19 + hk], in1=gqk[:, :], op0=ALU.mult, op1=ALU.mult),
                             [ps_kv2.b, stat_.b, gqk.b], [kn.b])
                    yield th
                    if full:
                        gate_pre(1)
                        yield th
                    ps_t2 = psum(th)
                    ptv2 = ps_t2[:, :].bitcast(BF16)
                    S.op("pe", lambda e: e.transpose(ptv2[:, 0:128], kn[:, :], identb[:, :]), [kn.b, CB.b], [ps_t2.b])
                    S.op("act", lambda e: e.activation(out=kTr[rs][:, :], in_=ptv2[:, 0:128], func=AF.Copy),
                         [ps_t2.b], [kTr[rs].b])
                if not full:
                    return
                ret = ret2[s]
                for half in range(2):
                    sg = sgt[half]
                    S.op("dve", lambda e, half=half, sg=sg: e.tensor_tensor(
                        out=ret[:, half * 512:(half + 1) * 512], in0=gnt[:, half * 512:(half + 1) * 512], in1=sg[:, :],
                        op=ALU.mult), [gnt.b, sg.b], [ret.b])
                    yield th

            def back(ci):
                j_own = ci - NPREV
                s = ci % 2
                x_s = xt[s]
                hT, ret, oTn = hT2[s], ret2[s], oTn2[s]
                def gate_pre(idx, col0):
                    ps_g = inproj("b", hT, col0, 512)
                    S.op("act", lambda e, ps_g=ps_g, idx=idx: e.activation(out=sgb[idx][:, :], in_=ps_g[:, :],
                                                                           func=AF.Tanh, scale=rr2[s][:, 1:2]),
                         [ps_g.b, rr2[s].b], [sgb[idx].b])

                rs = ci % 3
                rp = (ci - 1) % 3
                ps_sq = inproj("b", hT, O_SQ, 512)
                rr = rr2[s]
                S.op("act", lambda e: e.activation(out=sqq[:, :], in_=ps_sq[:, :], func=AF.Square, scale=rr[:, 0:1]),
                     [ps_sq.b, rr.b], [sqq.b])
                S.op("dve", lambda e: e.tensor_reduce(out=statb[:, 8:16],
                                                      in_=sqq[:, :].rearrange("p (h d) -> p h d", h=8),
                                                      axis=AX.X, op=ALU.add), [sqq.b], [statb.b])
                rms_rstd(statb[:, 8:16], statb[:, 20:28], 8, 1.0 / 64, [statb.b], statb.b)
                S.op("dve", lambda e: e.tensor_scalar(out=statb[:, 20:28], in0=statb[:, 20:28], scalar1=rr[:, 0:1],
                                                      scalar2=None, op0=ALU.mult), [statb.b, rr.b], [statb.b])
                S.op("dve", lambda e: e.tensor_tensor(
                    out=qn[:, :, :, :].rearrange("p i j d -> p j i d"),
                    in0=ps_sq[:, :].rearrange("p (j i d) -> p j i d", j=2, i=4),
                    in1=statb[:, 20:28].rearrange("p (j i) -> p j i", j=2).unsqueeze(3).broadcast_to([128, 2, 4, 64]),
                    op=ALU.mult), [ps_sq.b, statb.b], [qn.b])
                yield "b"
                gate_pre(0, O_GA)
                yield "b"
                gate_pre(1, O_GA + 512)
                yield "b"
                ps_t3 = psum("b")
                ptv3 = ps_t3[:, :].bitcast(BF16)
                for i in range(4):
                    S.op("pe", lambda e, i=i: e.transpose(ptv3[:, i * 128:(i + 1) * 128],
                                                          qn[:, i, :, :].rearrange("p j d -> p (j d)"),
                                                          identb[:, :]), [qn.b, CB.b], [ps_t3.b], signal=(i == 3))
                S.op("act", lambda e: e.activation(out=qnT[:, :, :].rearrange("p i t -> p (i t)"),
                                                   in_=ptv3[:, 0:512], func=AF.Copy), [ps_t3.b], [qnT.b])
                yield "b"
                first_own = (ci == NPREV)
                ebs = {}
                for jg in range(2):
                    for ch, (ring, msk) in enumerate(((rp, maskP0 if first_own else maskP), (rs, maskC))):
                        ps_sc = psum("b")
                        S.op("pe", lambda e, jg=jg, ring=ring, ps_sc=ps_sc: e.matmul(
                            ps_sc[:, :], lhsT=kTr[ring][jg * 64:(jg + 1) * 64, :],
                            rhs=qnT[jg * 64:(jg + 1) * 64, :, :].rearrange("p i t -> p (i t)"),
                            start=True, stop=True), [kTr[ring].b, qnT.b], [ps_sc.b])
                        ebuf = eb[eb_i[0] % 4]
                        eb_i[0] += 1
                        ebs[(jg, ch)] = ebuf
                        S.op("act", lambda e, ps_sc=ps_sc, ebuf=ebuf: e.activation(
                            out=ebuf[:, :], in_=ps_sc[:, :], func=AF.Exp, bias=negc[:, 0:1], scale=1.0),
                             [ps_sc.b, negc.b], [ebuf.b])
                        S.op("dve", lambda e, ebuf=ebuf, msk=msk: e.tensor_tensor(
                            out=ebuf[:, :].rearrange("p (i t) -> p i t", i=4),
                            in0=ebuf[:, :].rearrange("p (i t) -> p i t", i=4),
                            in1=msk[:, :].unsqueeze(1).broadcast_to([128, 4, 128]), op=ALU.mult),
                             [ebuf.b, msk.b], [ebuf.b])
                        yield "b"
                gate_pre(2, O_GB)
                yield "b"
                ps_pv = [psum("b"), psum("b")]
                for jg in range(2):
                    for i in range(4):
                        for ch, ring in enumerate((rp, rs)):
                            S.op("pe", lambda e, jg=jg, i=i, ch=ch, ring=ring: e.matmul(
                                ps_pv[jg][:, i * 65:(i + 1) * 65], lhsT=ebs[(jg, ch)][:, i * 128:(i + 1) * 128],
                                rhs=vsr[ring][:, jg, :], start=(ch == 0), stop=(ch == 1)),
                                 [vsr[ring].b, ebs[(jg, ch)].b], [ps_pv[jg].b], signal=(i == 3 and ch == 1))
                for jg in range(2):
                    pvw = ps_pv[jg][:, 0:260].rearrange("p (i c) -> p i c", i=4)
                    S.op("dve", lambda e, jg=jg, pvw=pvw: e.tensor_tensor(
                        out=statb[:, 32 + jg * 4:36 + jg * 4].unsqueeze(2), in0=pvw[:, :, 64:65],
                        in1=sinkexp[:, jg * 4:(jg + 1) * 4].unsqueeze(2), op=ALU.add),
                         [ps_pv[jg].b, sinkexp.b], [statb.b])
                    S.op("dve", lambda e, jg=jg: e.reciprocal(out=statb[:, 40 + jg * 4:44 + jg * 4],
                                                              in_=statb[:, 32 + jg * 4:36 + jg * 4]), [statb.b], [statb.b])
                    S.op("dve", lambda e, jg=jg, pvw=pvw: e.tensor_tensor(
                        out=on[:, jg * 4:(jg + 1) * 4, :], in0=pvw[:, :, 0:64],
                        in1=statb[:, 40 + jg * 4:44 + jg * 4].unsqueeze(2).broadcast_to([128, 4, 64]), op=ALU.mult),
                         [ps_pv[jg].b, statb.b], [on.b])
                yield "b"
                gate_pre(3, O_GB + 512)
                yield "b"
                ps = psum("b")
                transposes(lambda k: on[:, 2 * k:2 * k + 2, :].rearrange("p a d -> p (a d)"), 4, ps, [on.b])
                S.op("act", lambda e, ps=ps: e.activation(out=oTn[:, :, :].rearrange("p k t -> p (k t)"),
                                                          in_=ps[:, :].bitcast(BF16)[:, 0:512], func=AF.Copy),
                     [ps.b], [oTn.b])
                yield "b"
                ps = psum("b")
                transposes(lambda k: ret[:, k * 128:(k + 1) * 128], 8, ps, [ret.b])
                S.op("act", lambda e, ps=ps: e.activation(out=xT2[:, :, :].rearrange("p k t -> p (k t)"),
                                                          in_=ps[:, :].bitcast(BF16), func=AF.Copy), [ps.b], [xT2.b])
                yield "b"
                for half in range(2):
                    ps_y = psum("b")
                    for k in range(8):
                        S.op("pe", lambda e, k=k, ps_y=ps_y, half=half: e.matmul(
                            ps_y[:, :], lhsT=xT2[:, k, :], rhs=Wro[:, k, half * 512:(half + 1) * 512],
                            start=(k == 0), stop=(k == 7)), [xT2.b, Wro.b], [ps_y.b], signal=(k == 7))
                    sg = sgb[half]
                    S.op("dve", lambda e, ps_y=ps_y, sg=sg, half=half: e.scalar_tensor_tensor(
                        out=ma[:, half * 512:(half + 1) * 512], in0=sg[:, :], scalar=1.0, in1=ps_y[:, :],
                        op0=ALU.add, op1=ALU.mult), [ps_y.b, sg.b], [ma.b])
                    yield "b"
                for half in range(2):
                    ps_y = psum("b")
                    for k in range(4):
                        S.op("pe", lambda e, k=k, ps_y=ps_y, half=half: e.matmul(
                            ps_y[:, :], lhsT=oTn[:, k, :], rhs=Wso[:, k, half * 512:(half + 1) * 512],
                            start=(k == 0), stop=(k == 3)), [oTn.b, Wso.b], [ps_y.b], signal=(k == 3))
                    sg = sgb[2 + half]
                    S.op("dve", lambda e, ps_y=ps_y, sg=sg, half=half: e.scalar_tensor_tensor(
                        out=tb[:, half * 512:(half + 1) * 512], in0=sg[:, :], scalar=1.0, in1=ps_y[:, :],
                        op0=ALU.add, op1=ALU.mult), [ps_y.b, sg.b], [tb.b])
                    yield "b"
                S.op("dve", lambda e: e.tensor_tensor(out=tb[:, :], in0=tb[:, :], in1=ma[:, :], op=ALU.add),
                     [tb.b, ma.b], [tb.b])
                yield "b"
                ps = psum("b")
                transposes(lambda k: tb[:, k * 128:(k + 1) * 128], 8, ps, [tb.b])
                S.op("act", lambda e, ps=ps: e.activation(out=xT2[:, :, :].rearrange("p k t -> p (k t)"),
                                                          in_=ps[:, :].bitcast(BF16), func=AF.Copy), [ps.b], [xT2.b])
                yield "b"
                for half in range(2):
                    ps_y = psum("b")
                    for k in range(8):
                        S.op("pe", lambda e, k=k, ps_y=ps_y, half=half: e.matmul(
                            ps_y[:, :], lhsT=xT2[:, k, :], rhs=Wout[:, k, half * 512:(half + 1) * 512],
                            start=(k == 0), stop=(k == 7)), [xT2.b, Wout.b], [ps_y.b], signal=(k == 7))
                    S.op("dve", lambda e, ps_y=ps_y, half=half: e.tensor_tensor(
                        out=x_s[:, half * 512:(half + 1) * 512], in0=ps_y[:, :],
                        in1=x_s[:, half * 512:(half + 1) * 512], op=ALU.add), [ps_y.b, x_s.b], [x_s.b])
                    yield "b"
                S.dma("sp", st_x1[s], lambda e: e.dma_start(out=x1buf[j_own * 128:(j_own + 1) * 128, :], in_=x_s[:, :]),
                      reads=[x_s.b], writes=[x1_tiles[j_own]])
                S.op("act", lambda e: e.activation(out=h2[:, :], in_=x_s[:, :], func=AF.Square,
                                                   accum_out=statb[:, 0:1]), [x_s.b], [h2.b, statb.b])
                S.op("dve", lambda e: e.tensor_scalar(out=statb[:, 1:2], in0=statb[:, 0:1], scalar1=1.0 / D,
                                                       scalar2=EPS, op0=ALU.mult, op1=ALU.add), [statb.b], [statb.b])
                S.op("pool", lambda e: e.tensor_tensor(out=statb[:, 1:2], in0=statb[:, 1:2], in1=nhalf[:, 0:1],
                                                       op=ALU.pow), [statb.b, nhalf.b], [statb.b])
                S.op("dve", lambda e: e.tensor_scalar(out=h2[:, :], in0=x_s[:, :], scalar1=statb[:, 1:2],
                                                      scalar2=None, op0=ALU.mult), [x_s.b, statb.b], [h2.b])
                S.dma("sp", st_h2, lambda e: e.dma_start(out=h2buf[j_own * 128:(j_own + 1) * 128, :], in_=h2[:, :]),
                      reads=[h2.b], writes=[h2_tiles[j_own]])
                yield "b"
                ps_l = psum("b")
                for hf in range(2):
                    ps_a = psum("b")
                    for k4 in range(4):
                        k = hf * 4 + k4
                        S.op("pe", lambda e, k=k, k4=k4, ps_a=ps_a: e.transpose(
                            ps_a[:, k4 * 128:(k4 + 1) * 128], x_s[:, k * 128:(k + 1) * 128],
                            C[:, C_IDENT:C_IDENT + 128]), [x_s.b, C.b], [ps_a.b], signal=(k4 == 3))
                    S.op("act", lambda e, ps_a=ps_a: e.activation(out=x1T[:, :, :].rearrange("p k t -> p (k t)"),
                                                                  in_=ps_a[:, :], func=AF.Copy), [ps_a.b], [x1T.b])
                    for k4 in range(4):
                        k = hf * 4 + k4
                        S.op("pe", lambda e, k=k, k4=k4: e.matmul(ps_l[:, 0:36], lhsT=x1T[:, k4, :], rhs=wrs[:, k, :],
                                                                  start=(k == 0), stop=(k == 7)), [x1T.b, wrs.b],
                             [ps_l.b], signal=(k4 == 3))
                S.op("dve", lambda e: e.scalar_tensor_tensor(out=L[:, j_own, :], in0=ps_l[:, 0:36],
                                                             scalar=statb[:, 1:2], in1=C[:, C_BR:C_BR + 36],
                                                             op0=ALU.mult, op1=ALU.add), [ps_l.b, statb.b, C.b], [L.b])
                yield "b"

            def step(g, last):
                try:
                    last[0] = next(g)
                    release(last[0])
                    return True
                except StopIteration:
                    if last[0] is not None:
                        release(last[0])
                    return False

            def run_all(g):
                last = [None]
                while step(g, last):
                    pass

            setB = (ma, TV(xT2.t[:, 0:4, :].rearrange("p h (t d) -> p h t d", t=2), xT2.b),
                    [TV(eb[i].t[:, 0:256].rearrange("p (h d) -> p h d", h=4), eb[i].b) for i in range(4)],
                    tb, TV(xT2.t[:, 4:8, :], xT2.b), statp, "b")

            def interleave(gens):
                gens = [(g, [None]) for g in gens]
                while gens:
                    for item in list(gens):
                        if not step(item[0], item[1]):
                            gens.remove(item)

            load_tile(0)
            if NT > 1:
                load_tile(1)
            ci = 0
            folded = False
            zfilled = False
            while ci < NPREV:
                if wq:
                    issue_weights(len(wq) if ci >= NPREV - 2 else 3)
                if ci >= 4 and not zfilled:
                    zero_fill()
                    zfilled = True
                if not wq and ci < NPREV - 1 and not folded:
                    fold_rest()
                    folded = True
                if ci + 1 < NPREV:
                    interleave([front(ci), front(ci + 1, setB)])
                    done = 2
                else:
                    interleave([front(ci)])
                    done = 1
                for c2 in range(ci + 2, ci + 2 + done):
                    if c2 < NT:
                        load_tile(c2)
                ci += done
            if not zfilled:
                zero_fill()
            if wq:
                issue_weights(len(wq))
            if not folded:
                fold_rest()
            if NPREV % 2 == 1 and NPREV + 1 < NT:
                pass
            interleave([front(NPREV)])
            step_counts = {}

            def interleave_prop(named):
                live = {n: (g, [None]) for n, g in named}
                done = {n: 0 for n, _ in named}
                while live:
                    n = min(live, key=lambda k: (done[k] + 1) / float(step_counts.get(k, 1e9)))
                    if ORDER is not None and len(live) == 2:
                        pos = done["front"] + done["back"]
                        if pos < len(ORDER):
                            n = "front" if ORDER[pos] == "f" else "back"
                    if step(live[n][0], live[n][1]):
                        done[n] += 1
                    else:
                        del live[n]
                LAST_COUNTS.update(step_counts)

            for ci in range(NPREV, NT):
                named = []
                if ci + 1 < NT:
                    named.append(("front", front(ci + 1)))
                named.append(("back", back(ci)))
                if len(named) == 2 and "front" not in step_counts:
                    live = {n: (g, [None]) for n, g in named}
                    cnt = {n: 0 for n in live}
                    while live:
                        for n in list(live):
                            if step(live[n][0], live[n][1]):
                                cnt[n] += 1
                            else:
                                del live[n]
                    step_counts.update(cnt)
                elif len(named) == 2:
                    interleave_prop(named)
                else:
                    interleave([g for _, g in named])
                if ci + 2 < NT:
                    load_tile(ci + 2)
            S.barrier()

        if not moe:
            with contextlib.ExitStack() as p2:
                tt = [T(p2.enter_context(nc.sbuf_tensor("cp%d" % i, [128, D], F32)), "cp%d" % i) for i in range(2)]
                st_l = [S.stream("cpl%d" % i) for i in range(2)]
                st_o = S.stream("cpo")
                for j in range(NOWN):
                    t = tt[j % 2]
                    S.dma("sp", st_l[j % 2], lambda e, j=j, t=t: e.dma_start(out=t[:, :], in_=x1buf[j * 128:(j + 1) * 128, :]),
                          writes=[t.b])
                    S.dma("sp", st_o, lambda e, j=j, t=t: e.dma_start(out=y[j * 128:(j + 1) * 128, :], in_=t[:, :]),
                          reads=[t.b])
                S.barrier()
            S.emit()
            return nc

        NJ = NOWN
        with contextlib.ExitStack() as p2:
            def sb2(name, shape, dt):
                return T(p2.enter_context(nc.sbuf_tensor("sb_" + name, list(shape), dt)), name)

            pass
        p23 = stack
        wg = [sb("wg%d" % i, [128, 8, DFF], BF16) for i in range(2)]
        wu = [sb("wu%d" % i, [128, 8, DFF], BF16) for i in range(2)]
        wd = [sb("wd%d" % i, [128, 4, D], BF16) for i in range(2)]
        st_we = [S.stream("we%d" % i) for i in range(2)]

        def load_w(ex):
            s = ex % 2
            S.dma("pool", st_we[s], lambda e: e.dma_start(out=wg[s][:, :, :],
                                                           in_=w_gate[ex].rearrange("(k p) n -> p k n", p=128)),
                  writes=[wg[s].b])
            S.dma("pool", st_we[s], lambda e: e.dma_start(out=wu[s][:, :, :],
                                                           in_=w_up[ex].rearrange("(k p) n -> p k n", p=128)),
                  writes=[wu[s].b])
            S.dma("pool", st_we[s], lambda e: e.dma_start(out=wd[s][:, :, :],
                                                           in_=w_down[ex].rearrange("(k p) n -> p k n", p=128)),
                  writes=[wd[s].b])
            S.seal(st_we[s], [wg[s].b, wu[s].b, wd[s].b])


        load_w(0)
        load_w(1)
        regcache = {}

        def bc_reg(e):
            if "bc" not in regcache:
                regcache["bc"] = e.to_reg(NSLOT - 1)
            return regcache["bc"]

        slot_i = sb("slot_i", [128, 2, NJ], I32)
        wts = sb("wts", [128, 2, NJ], F32)
        with contextlib.ExitStack() as p2:
            def sb2(name, shape, dt):
                return T(p2.enter_context(nc.sbuf_tensor("sb_" + name, list(shape), dt)), name)

            gmax = sb2("gmax", [128, NJ], F32)
            ohg = sb2("ohg", [128, NJ, 4], F32)
            ge = sb2("ge", [128, NJ, 4], F32)
            gsum = sb2("gsum", [128, NJ], F32)
            gprob = sb2("gprob", [128, NJ], F32)
            elm = sb2("elm", [128, NJ, 4, 8], F32)
            sel = sb2("sel", [128, NJ, 8], F32)
            m1 = sb2("m1", [128, NJ], F32)
            m2 = sb2("m2", [128, NJ], F32)
            oh = [sb2("oh%d" % i, [128, NJ, 8], F32) for i in range(2)]
            sel2 = sb2("sel2", [128, NJ, 8], F32)
            Ek = [sb2("E%d" % i, [128, NJ, 4, 8], F32) for i in range(2)]
            M = sb2("M", [128, NJ, 32], F32)
            pos = sb2("pos", [128, NJ, 32], F32)
            base = sb2("base", [128, NJ, 32], F32)
            tot = sb2("tot", [128, NJ, 32], F32)
            ones_f = sb2("ones_f", [128, 128], F32)
            tmp = sb2("tmp", [128, NJ, 32], F32)
            slot_f = sb2("slot_f", [128, 2, NJ], F32)
            dd = sb2("dd", [128, NJ], F32)

            V = "dve"
            Lg = L[:, :, 0:4]
            Le = L[:, :, 4:36]
            S.op(V, lambda e: e.memset(ones_f[:, :], 1.0), [], [ones_f.b])
            S.op(V, lambda e: e.tensor_reduce(out=gmax[:, :], in_=Lg, axis=AX.X, op=ALU.max), [L.b], [gmax.b])
            S.op(V, lambda e: e.tensor_tensor(out=ohg[:, :, :], in0=Lg,
                                              in1=gmax[:, :].unsqueeze(2).broadcast_to([128, NJ, 4]),
                                              op=ALU.is_equal), [L.b, gmax.b], [ohg.b])
            S.op(V, lambda e: e.tensor_tensor(out=ge[:, :, :], in0=Lg,
                                              in1=gmax[:, :].unsqueeze(2).broadcast_to([128, NJ, 4]),
                                              op=ALU.subtract), [L.b, gmax.b], [ge.b])
            S.op("act", lambda e: e.activation(out=ge[:, :, :], in_=ge[:, :, :], func=AF.Exp), [ge.b], [ge.b])
            S.op(V, lambda e: e.tensor_reduce(out=gsum[:, :], in_=ge[:, :, :], axis=AX.X, op=ALU.add), [ge.b], [gsum.b])
            S.op(V, lambda e: e.reciprocal(out=gprob[:, :], in_=gsum[:, :]), [gsum.b], [gprob.b])
            S.op(V, lambda e: e.tensor_tensor(out=elm[:, :, :, :], in0=Le.rearrange("p j (g x) -> p j g x", g=4),
                                              in1=ohg[:, :, :].unsqueeze(3).broadcast_to([128, NJ, 4, 8]),
                                              op=ALU.mult), [L.b, ohg.b], [elm.b])
            S.op(V, lambda e: e.tensor_reduce(out=sel[:, :, :], in_=elm[:, :, :, :].rearrange("p j g x -> p j x g"),
                                              axis=AX.X, op=ALU.add), [elm.b], [sel.b])
            S.op(V, lambda e: e.tensor_reduce(out=m1[:, :], in_=sel[:, :, :], axis=AX.X, op=ALU.max), [sel.b], [m1.b])
            S.op(V, lambda e: e.tensor_tensor(out=oh[0][:, :, :], in0=sel[:, :, :],
                                              in1=m1[:, :].unsqueeze(2).broadcast_to([128, NJ, 8]),
                                              op=ALU.is_equal), [sel.b, m1.b], [oh[0].b])
            S.op(V, lambda e: e.scalar_tensor_tensor(out=sel2[:, :, :], in0=oh[0][:, :, :], scalar=-1e30,
                                                     in1=sel[:, :, :], op0=ALU.mult, op1=ALU.add),
                 [oh[0].b, sel.b], [sel2.b])
            S.op(V, lambda e: e.tensor_reduce(out=m2[:, :], in_=sel2[:, :, :], axis=AX.X, op=ALU.max), [sel2.b], [m2.b])
            S.op(V, lambda e: e.tensor_tensor(out=oh[1][:, :, :], in0=sel2[:, :, :],
                                              in1=m2[:, :].unsqueeze(2).broadcast_to([128, NJ, 8]),
                                              op=ALU.is_equal), [sel2.b, m2.b], [oh[1].b])
            S.op(V, lambda e: e.tensor_tensor(out=dd[:, :], in0=m2[:, :], in1=m1[:, :], op=ALU.subtract),
                 [m1.b, m2.b], [dd.b])
            S.op("act", lambda e: e.activation(out=dd[:, :], in_=dd[:, :], func=AF.Exp), [dd.b], [dd.b])
            S.op(V, lambda e: e.tensor_scalar(out=dd[:, :], in0=dd[:, :], scalar1=1.0, scalar2=None, op0=ALU.add),
                 [dd.b], [dd.b])
            S.op(V, lambda e: e.reciprocal(out=dd[:, :], in_=dd[:, :]), [dd.b], [dd.b])
            S.op(V, lambda e: e.tensor_scalar(out=gprob[:, :], in0=gprob[:, :], scalar1=0.5, scalar2=None,
                                              op0=ALU.mult), [gprob.b], [gprob.b])
            S.op(V, lambda e: e.tensor_tensor(out=wts[:, 0, :], in0=dd[:, :], in1=gprob[:, :], op=ALU.mult),
                 [dd.b, gprob.b], [wts.b])
            S.op(V, lambda e: e.tensor_tensor(out=wts[:, 1, :], in0=gprob[:, :], in1=wts[:, 0, :], op=ALU.subtract),
                 [gprob.b, wts.b], [wts.b])
            for kk in range(2):
                S.op(V, lambda e, kk=kk: e.tensor_tensor(
                    out=Ek[kk][:, :, :, :], in0=ohg[:, :, :].unsqueeze(3).broadcast_to([128, NJ, 4, 8]),
                    in1=oh[kk][:, :, :].unsqueeze(2).broadcast_to([128, NJ, 4, 8]), op=ALU.mult),
                     [ohg.b, oh[kk].b], [Ek[kk].b])
            S.op(V, lambda e: e.tensor_tensor(out=M[:, :, :], in0=Ek[0][:, :, :, :].rearrange("p j g x -> p j (g x)"),
                                              in1=Ek[1][:, :, :, :].rearrange("p j g x -> p j (g x)"), op=ALU.add),
                 [Ek[0].b, Ek[1].b], [M.b])
            NCOL = NJ * 32
            Mf = M[:, :, :].rearrange("p j x -> p (j x)")
            for c0 in range(0, NCOL, 512):
                cw = min(512, NCOL - c0)
                ps1 = psum()
                S.op("pe", lambda e, c0=c0, cw=cw, ps1=ps1: e.matmul(ps1[:, 0:cw], lhsT=C[:, C_U:C_U + 128],
                                                                     rhs=Mf[:, c0:c0 + cw], start=True, stop=True),
                     [C.b, M.b], [ps1.b])
                S.op(V, lambda e, c0=c0, cw=cw, ps1=ps1: e.tensor_copy(
                    out=pos[:, :, :].rearrange("p j x -> p (j x)")[:, c0:c0 + cw], in_=ps1[:, 0:cw]),
                     [ps1.b], [pos.b])
                ps2 = psum()
                S.op("pe", lambda e, c0=c0, cw=cw, ps2=ps2: e.matmul(ps2[:, 0:cw], lhsT=ones_f[:, :],
                                                                     rhs=Mf[:, c0:c0 + cw], start=True, stop=True),
                     [ones_f.b, M.b], [ps2.b])
                S.op(V, lambda e, c0=c0, cw=cw, ps2=ps2: e.tensor_copy(
                    out=tot[:, :, :].rearrange("p j x -> p (j x)")[:, c0:c0 + cw], in_=ps2[:, 0:cw]),
                     [ps2.b], [tot.b])
            S.op(V, lambda e: e.tensor_copy(out=base[:, 0, :], in_=C[:, C_EC:C_EC + 32]), [C.b], [base.b])
            for j in range(1, NJ):
                S.op(V, lambda e, j=j: e.tensor_tensor(out=base[:, j, :], in0=base[:, j - 1, :], in1=tot[:, j - 1, :],
                                                       op=ALU.add), [base.b, tot.b], [base.b])
            S.op(V, lambda e: e.tensor_tensor(out=pos[:, :, :], in0=pos[:, :, :], in1=base[:, :, :], op=ALU.add),
                 [pos.b, base.b], [pos.b])
            S.op(V, lambda e: e.tensor_tensor(out=tmp[:, :, :], in0=pos[:, :, :],
                                              in1=C[:, C_EC:C_EC + 32].unsqueeze(1).broadcast_to([128, NJ, 32]),
                                              op=ALU.subtract), [pos.b, C.b], [tmp.b])
            S.op(V, lambda e: e.tensor_scalar(out=tmp[:, :, :], in0=tmp[:, :, :], scalar1=float(CAP) - 0.5,
                                              scalar2=1.0e6, op0=ALU.is_gt, op1=ALU.mult), [tmp.b], [tmp.b])
            S.op(V, lambda e: e.tensor_tensor(out=pos[:, :, :], in0=pos[:, :, :], in1=tmp[:, :, :], op=ALU.add),
                 [pos.b, tmp.b], [pos.b])
            for kk in range(2):
                S.op(V, lambda e, kk=kk: e.tensor_tensor(out=tmp[:, :, :],
                                                         in0=Ek[kk][:, :, :, :].rearrange("p j g x -> p j (g x)"),
                                                         in1=pos[:, :, :], op=ALU.mult), [Ek[kk].b, pos.b], [tmp.b])
                S.op(V, lambda e, kk=kk: e.tensor_reduce(out=slot_f[:, kk, :], in_=tmp[:, :, :], axis=AX.X,
                                                         op=ALU.add), [tmp.b], [slot_f.b])
            S.op(V, lambda e: e.tensor_copy(out=slot_i[:, :, :], in_=slot_f[:, :, :]), [slot_f.b], [slot_i.b])

            for b_ in h2_tiles:
                b_.w = Tok(st_h2.sem, st_h2.count)
            gfb = sb2("gfb", [128, D], F32)
            st_gf = S.stream("gffn")
            S.dma("sp", st_gf, lambda e: e.dma_start(out=gfb[:, :], in_=gffn.broadcast_to([128, D])), writes=[gfb.b])
            NHB = 8
            hb = [sb2("hb%d" % i, [128, D], BF16) for i in range(NHB)]
            st_hb = [S.stream("hb%d" % i) for i in range(NHB)]
            st_sc = [S.stream("scat%d" % i) for i in range(NHB)]
            xs_all = Buf("xs_all")
            for j in range(NJ):
                t = hb[j % NHB]
                S.dma("sp", st_hb[j % NHB], lambda e, j=j, t=t: e.dma_start(out=t[:, :], in_=h2buf[j * 128:(j + 1) * 128, :]),
                      reads=[h2_tiles[j]], writes=[t.b])
                S.op("dve", lambda e, t=t: e.tensor_tensor(
                    out=t[:, :], in0=t[:, :], in1=gfb[:, :], op=ALU.mult), [t.b, gfb.b], [t.b])
                for kk in range(2):
                    S.dma("pool", st_sc[j % NHB], lambda e, j=j, kk=kk, t=t: e.indirect_dma_start(
                        out=xsbuf[0:NSLOT, :], out_offset=bass.IndirectOffsetOnAxis(ap=slot_i[:, kk, j:j + 1], axis=0),
                        in_=t[:, :], in_offset=None, bounds_check=bc_reg(e), oob_is_err=False),
                          reads=[t.b, slot_i.b], writes=[])
            S.barrier()

        NB = CAP // 128
        st_ys = [S.stream("ys%d" % i) for i in range(2)]
        with contextlib.ExitStack() as p3:
            def sb3(name, shape, dt):
                return T(p3.enter_context(nc.sbuf_tensor("sb_" + name, list(shape), dt)), name)

            xb = [sb3("xb%d" % i, [128, D], BF16) for i in range(3)]
            st_xb = [S.stream("xb%d" % i) for i in range(3)]
            xT = [sb3("xTe%d" % i, [128, 8, CAP], BF16) for i in range(2)]
            gs = [sb3("gs%d" % i, [128, CAP], BF16) for i in range(2)]
            aT = [sb3("aT%d" % i, [128, 4, CAP], BF16) for i in range(2)]
            yt = [sb3("yt%d" % i, [128, D], BF16) for i in range(2)]
            xb_i = [0]
            gs_i = [0]
            yt_i = [0]

            def load_x(ex):
                res = []
                for bi in range(NB):
                    t = xb[xb_i[0] % 3]
                    stx = st_xb[xb_i[0] % 3]
                    xb_i[0] += 1
                    r0 = ex * CAP + bi * 128
                    S.dma("sp", stx, lambda e, t=t, r0=r0: e.dma_start(out=t[:, :], in_=xsbuf[r0:r0 + 128, :]),
                          writes=[t.b])
                    res.append(t)
                return res

            def do_transposes(ex):
                xbl = load_x(ex)
                xTe = xT[ex % 2]
                for bi in range(NB):
                    ps = psum()
                    t = xbl[bi]
                    transposes(lambda k, t=t: t[:, k * 128:(k + 1) * 128], 8, ps, [t.b])
                    pv = ps[:, :].bitcast(BF16).rearrange("p (k t) -> p k t", k=8)
                    if bi % 2 == 0:
                        S.op("act", lambda e, pv=pv, bi=bi, xTe=xTe: e.activation(
                            out=xTe[:, :, bi * 128:(bi + 1) * 128], in_=pv, func=AF.Copy), [ps.b, t.b], [xTe.b])
                    else:
                        S.op("dve", lambda e, pv=pv, bi=bi, xTe=xTe: e.tensor_copy(
                            out=xTe[:, :, bi * 128:(bi + 1) * 128], in_=pv), [ps.b, t.b], [xTe.b])

            def gate_up(ex):
                s = ex % 2
                xTe = xT[s]
                aTe = aT[s]
                for f in range(4):
                    ps_g = psum()
                    for k in range(8):
                        S.op("pe", lambda e, k=k, f=f, ps_g=ps_g: e.matmul(
                            ps_g[:, 0:CAP], lhsT=wg[s][:, k, f * 128:(f + 1) * 128], rhs=xTe[:, k, :],
                            start=(k == 0), stop=(k == 7)), [wg[s].b, xTe.b], [ps_g.b], signal=(k == 7))
                    ps_u = psum()
                    for k in range(8):
                        S.op("pe", lambda e, k=k, f=f, ps_u=ps_u: e.matmul(
                            ps_u[:, 0:CAP], lhsT=wu[s][:, k, f * 128:(f + 1) * 128], rhs=xTe[:, k, :],
                            start=(k == 0), stop=(k == 7)), [wu[s].b, xTe.b], [ps_u.b], signal=(k == 7))
                    g_ = gs[gs_i[0] % 2]
                    gs_i[0] += 1
                    S.op("act", lambda e, ps_g=ps_g, g_=g_: e.activation(out=g_[:, :], in_=ps_g[:, 0:CAP], func=AF.Tanh,
                                                                         scale=0.5), [ps_g.b], [g_.b])
                    S.op("dve", lambda e, ps_g=ps_g, g_=g_: e.scalar_tensor_tensor(
                        out=g_[:, :], in0=g_[:, :], scalar=1.0, in1=ps_g[:, 0:CAP], op0=ALU.add, op1=ALU.mult),
                         [ps_g.b, g_.b], [g_.b])
                    S.op("dve", lambda e, ps_u=ps_u, g_=g_, f=f: e.tensor_tensor(out=aTe[:, f, :], in0=ps_u[:, 0:CAP],
                                                                                 in1=g_[:, :], op=ALU.mult),
                         [ps_u.b, g_.b], [aTe.b])

            def down(ex):
                s = ex % 2
                aTe = aT[s]
                for bi in range(NB):
                    y_ = yt[yt_i[0] % 2]
                    st_y = st_ys[yt_i[0] % 2]
                    yt_i[0] += 1
                    for half in range(2):
                        ps_y = psum()
                        for f in range(4):
                            S.op("pe", lambda e, f=f, bi=bi, half=half, ps_y=ps_y: e.matmul(
                                ps_y[:, :], lhsT=aTe[:, f, bi * 128:(bi + 1) * 128],
                                rhs=wd[s][:, f, half * 512:(half + 1) * 512], start=(f == 0), stop=(f == 3)),
                                 [aTe.b, wd[s].b], [ps_y.b], signal=(f == 3))
                        if half == 0:
                            S.op("act", lambda e, ps_y=ps_y, y_=y_: e.activation(out=y_[:, 0:512], in_=ps_y[:, :],
                                                                                 func=AF.Copy), [ps_y.b], [y_.b])
                        else:
                            S.op("dve", lambda e, ps_y=ps_y, y_=y_: e.tensor_copy(out=y_[:, 512:1024], in_=ps_y[:, :]),
                                 [ps_y.b], [y_.b])
                    r0 = ex * CAP + bi * 128
                    S.dma("sp", st_y, lambda e, y_=y_, r0=r0: e.dma_start(out=ysbuf[r0:r0 + 128, :], in_=y_[:, :]),
                          reads=[y_.b])

            do_transposes(0)
            for ex in range(NE):
                gate_up(ex)
                if ex + 1 < NE:
                    do_transposes(ex + 1)
                down(ex)
                if ex + 2 < NE:
                    load_w(ex + 2)
            S.barrier()

        for b_ in x1_tiles:
            b_.w = Tok(st_x1[b_.w.sem is st_x1[1].sem].sem, st_x1[b_.w.sem is st_x1[1].sem].count)
        with contextlib.ExitStack() as p4:
            def sb4(name, shape, dt):
                return T(p4.enter_context(nc.sbuf_tensor("sb_" + name, list(shape), dt)), name)

            NSL = 8
            xl = [sb4("xl%d" % i, [128, D], F32) for i in range(NSL)]
            g1 = [sb4("g1%d" % i, [128, D], BF16) for i in range(NSL)]
            g2 = [sb4("g2%d" % i, [128, D], BF16) for i in range(NSL)]
            st_xl = [S.stream("xl%d" % i) for i in range(NSL)]
            st_g = [S.stream("g%d" % i) for i in range(NSL)]
            st_out = [S.stream("out%d" % i) for i in range(NSL)]

            def fetch(j):
                s = j % NSL
                S.dma("sp", st_xl[s], lambda e, j=j, s=s: e.dma_start(out=xl[s][:, :], in_=x1buf[j * 128:(j + 1) * 128, :]),
                      reads=[x1_tiles[j]], writes=[xl[s].b])
                for kk, gt_ in enumerate((g1[s], g2[s])):
                    S.dma("pool", st_g[s], lambda e, j=j, kk=kk, gt_=gt_: e.indirect_dma_start(
                        out=gt_[:, :], out_offset=None, in_=ysbuf[0:NSLOT, :],
                        in_offset=bass.IndirectOffsetOnAxis(ap=slot_i[:, kk, j:j + 1], axis=0),
                        bounds_check=bc_reg(e), oob_is_err=False), reads=[slot_i.b], writes=[gt_.b])
                S.seal(st_g[s], [g1[s].b, g2[s].b])

            for j in range(min(NSL - 1, NJ)):
                fetch(j)
            for j in range(NJ):
                s = j % NSL
                if j + NSL - 1 < NJ:
                    fetch(j + NSL - 1)
                S.op("dve", lambda e, j=j, s=s: e.scalar_tensor_tensor(out=xl[s][:, :], in0=g1[s][:, :],
                                                                       scalar=wts[:, 0, j:j + 1], in1=xl[s][:, :],
                                                                       op0=ALU.mult, op1=ALU.add),
                     [g1[s].b, wts.b, xl[s].b], [xl[s].b])
                S.op("dve", lambda e, j=j, s=s: e.scalar_tensor_tensor(out=xl[s][:, :], in0=g2[s][:, :],
                                                                       scalar=wts[:, 1, j:j + 1], in1=xl[s][:, :],
                                                                       op0=ALU.mult, op1=ALU.add),
                     [g2[s].b, wts.b, xl[s].b], [xl[s].b])
                S.dma("sp", st_out[s], lambda e, j=j, s=s: e.dma_start(out=y[j * 128:(j + 1) * 128, :], in_=xl[s][:, :]),
                      reads=[xl[s].b])
            S.barrier()
        S.emit()
    return nc


def make_consts(CAP):
    gam = np.array([1.0 - 2.0 ** (-5.0 - h) for h in range(NH)], dtype=np.float64)
    c = np.zeros((128, NCST), dtype=np.float32)
    idx = np.arange(128)
    c[:, C_IDENT:C_IDENT + 128] = np.eye(128, dtype=np.float32)
    kk = idx[:, None]
    qq = idx[None, :]
    dtp = np.zeros((128, 4, 128), dtype=np.float64)
    dq = np.zeros((128, 4, 128), dtype=np.float64)
    for h in range(NH):
        dtp[:, h, :] = (gam[h] ** (-(kk + 1.0))) * (DK ** -0.5) * (qq >= kk)
        dq[:, h, :] = gam[h] ** (qq + 1.0)
        c[:, C_KDEC + h] = (gam[h] ** (127.0 - idx)) * (DK ** -0.5)
    c[:, C_DTP:C_DTP + 512] = dtp.reshape(128, 512)
    c[:, C_DQROW:C_DQROW + 512] = dq.reshape(128, 512)
    c[:, C_U:C_U + 128] = (kk < qq).astype(np.float32)
    c[:, C_EC:C_EC + 32] = (np.arange(32) * CAP)[None, :]
    return c


def rope_tables(pos):
    half = 64
    inv = (np.float32(10000.0) ** (-(np.arange(half, dtype=np.float32)) / np.float32(half))).astype(np.float32)
    ang = pos.astype(np.float32)[:, None] * inv[None, :]
    return np.concatenate([np.cos(ang), np.sin(ang)], axis=1).astype(np.float32)


def prep_core(inputs, b, half, NPREV, NOWN, CAP, seq_own0=None):
    x = inputs["x"]
    n_prev = NPREV * 128
    n_own = NOWN * 128
    own0 = half * n_own if seq_own0 is None else seq_own0
    xc = np.zeros((n_prev + n_own, D), dtype=np.float32)
    cs = np.zeros((n_prev + n_own, 128), dtype=np.float32)
    if own0 > 0:
        xc[:n_prev] = x[b, own0 - n_prev:own0]
        cs[:n_prev] = rope_tables(np.arange(own0 - n_prev, own0))
    xc[n_prev:] = x[b, own0:own0 + n_own]
    cs[n_prev:] = rope_tables(np.arange(own0, own0 + n_own))
    c = make_consts(CAP)
    idx = np.arange(128)
    cb = np.zeros((128, 512), dtype=np.float32)
    cb[:, B_IDENT:B_IDENT + 128] = np.eye(128, dtype=np.float32)
    cb[:, B_MASKC:B_MASKC + 128] = (idx[None, :] >= idx[:, None]).astype(np.float32)
    cb[:, B_MASKP:B_MASKP + 128] = (idx[:, None] > idx[None, :]).astype(np.float32)
    if own0 > 0:
        cb[:, B_MASKP0:B_MASKP0 + 128] = (idx[:, None] > idx[None, :]).astype(np.float32)
    c[:, C_GMIX:C_GMIX + 8] = inputs["norm_mix_g"][0].reshape(8, 128).T
    c[:, C_GN:C_GN + 8] = inputs["ret_gn_g"][0].reshape(8, 128).T
    c[:, C_GFFN:C_GFFN + 8] = inputs["norm_ffn_g"][0].reshape(8, 128).T
    c[:, C_GQ:C_GQ + 64] = inputs["q_norm_g"][0][None, :]
    c[:, C_GK:C_GK + 64] = inputs["k_norm_g"][0][None, :]
    c[:, C_SINK:C_SINK + 8] = inputs["sinks"][0][None, :]
    c[:, C_BR:C_BR + 4] = inputs["b_router_group"][0][None, :]
    c[:, C_BR + 4:C_BR + 36] = inputs["b_router_expert"][0][None, :]
    return {"xc": xc, "cs": cs, "cst": c, "cstb": cb,
            "gffn": np.ascontiguousarray(inputs["norm_ffn_g"][0][None, :])}


def shared_inputs(inputs):
    wr = np.concatenate([inputs["w_router_group"][0], inputs["w_router_expert"][0]], axis=1)
    return {
        "w_in": np.ascontiguousarray(inputs["w_in"][0]),
        "w_ret_o": np.ascontiguousarray(inputs["w_ret_o"][0]),
        "w_swa_o": np.ascontiguousarray(inputs["w_swa_o"][0]),
        "w_out": np.ascontiguousarray(inputs["w_out"][0]),
        "wr": np.ascontiguousarray(wr),
        "w_gate": np.ascontiguousarray(inputs["w_gate"][0]),
        "w_up": np.ascontiguousarray(inputs["w_up"][0]),
        "w_down": np.ascontiguousarray(inputs["w_down"][0]),
    }


CAP_DEFAULT = 384
ORDER = "fbbfbbfbbbffbffbbbfbbfbbbfbbffbbbbfbf"
LAST_COUNTS = {}


def kernel(**inputs):
    inputs = {k: np.asarray(v) for k, v in inputs.items()}
    B, SEQ, _ = inputs["x"].shape
    NOWN = SEQ // 2 // 128
    NPREV = NOWN
    nc = build(NPREV=NPREV, NOWN=NOWN, CAP=CAP_DEFAULT, moe=True)
    sh = shared_inputs(inputs)
    in_maps = []
    for core in range(8):
        b, half = core // 2, core % 2
        m = prep_core(inputs, b, half, NPREV, NOWN, CAP_DEFAULT)
        m.update(sh)
        in_maps.append(m)
    res = run_bass_kernel_spmd(nc, in_maps, core_ids=list(range(8)))
    out = np.zeros((B, SEQ, D), dtype=np.float32)
    n_own = NOWN * 128
    for core in range(8):
        b, half = core // 2, core % 2
        out[b, half * n_own:(half + 1) * n_own] = res.results[core]["y"]
    return out
```
